# Optimizing a Trainium2 kernel written in Bass

```python
import jax, jax.numpy as jnp
from jax import lax
import numpy as np

D_MODEL = 1024
BATCH = 8
SEQ = 2048
DEPTH = 4

N_GROUPS = 4
GROUP_WIDTH = D_MODEL // N_GROUPS
HEADS = 4
RWKV_HEAD = GROUP_WIDTH // HEADS
RWKV_DECAY_RANK = 32
RWKV_A_RANK = 32
RWKV_GATE_RANK = 64
RWKV_GN_EPS = 64e-5
GLA_KEY_WIDTH = GROUP_WIDTH // 2
GLA_HEAD_K = GLA_KEY_WIDTH // HEADS
GLA_HEAD_V = GROUP_WIDTH // HEADS
GLA_GATE_RANK = 16
GLA_GATE_TAU = 16.0
LRU_CONV = 4
LRU_BLOCK = GROUP_WIDTH // HEADS
LRU_C = 8.0
HGRN_HEAD = GROUP_WIDTH // HEADS
CHUNK = 32
FFN_HIDDEN = -(-8 * D_MODEL // (3 * 256)) * 256
NORM_EPS = 1e-6

RWKV_COLS = 3 * GROUP_WIDTH + RWKV_DECAY_RANK + RWKV_A_RANK + RWKV_GATE_RANK
GLA_COLS = 2 * GLA_KEY_WIDTH + GROUP_WIDTH + GLA_GATE_RANK + GROUP_WIDTH
LRU_COLS = 2 * GROUP_WIDTH
HGRN_COLS = 4 * GROUP_WIDTH
IN_COLS = RWKV_COLS + GLA_COLS + LRU_COLS + HGRN_COLS

kernel_name = 'hymba_style_rwkv7_gla_rglru_hgrn2_trunk'


def _split(t, sizes):
    idx, acc = [], 0
    for s in sizes[:-1]:
        acc += s
        idx.append(acc)
    return jnp.split(t, idx, axis=-1)


def _rms_norm(x, g):
    xf = x.astype(jnp.float32)
    y = xf * lax.rsqrt(jnp.mean(xf * xf, axis=-1, keepdims=True) + NORM_EPS)
    return (y * g.astype(jnp.float32)).astype(x.dtype)


def _heads(t, h):
    b, s, c = t.shape
    return t.reshape(b, s, h, c // h)


def _rwkv7_scan(r, w, k, v, kk, a):
    b, t, h, n = r.shape
    xs = tuple(jnp.moveaxis(z, 1, 0) for z in (r, w, k, v, kk, a))

    def step(S, inp):
        r_t, w_t, k_t, v_t, kk_t, a_t = inp
        sa = jnp.einsum('bhvk,bhk->bhv', S, -kk_t)
        S = (S * w_t[:, :, None, :] + sa[..., None] * (kk_t * a_t)[:, :, None, :]
             + v_t[..., None] * k_t[:, :, None, :])
        return S, jnp.einsum('bhvk,bhk->bhv', S, r_t)

    S0 = jnp.zeros((b, h, n, n), jnp.float32)
    _, y = lax.scan(step, S0, xs)
    return jnp.moveaxis(y, 0, 1)


def _rwkv7_group(p, mu, w0, w_up, a0, a_up, g_up, k_k, k_a, r_k, ln_w, ln_b):
    p = p.astype(jnp.float32)
    prev = jnp.pad(p, ((0, 0), (1, 0), (0, 0)))[:, :-1]
    u = p + (prev - p) * mu
    r, k, v, w_low, a_low, g_low = _split(u, (GROUP_WIDTH, GROUP_WIDTH, GROUP_WIDTH,
                                             RWKV_DECAY_RANK, RWKV_A_RANK, RWKV_GATE_RANK))
    w_log = -jax.nn.softplus(-(w0 + jnp.tanh(w_low) @ w_up)) - 0.5
    decay = jnp.exp(-jnp.exp(w_log))
    a = jax.nn.sigmoid(a0 + a_low @ a_up)
    g = jax.nn.sigmoid(g_low) @ g_up
    r, k, v, decay, a = (_heads(z, HEADS) for z in (r, k, v, decay, a))
    kk = _heads(_heads(k.reshape(k.shape[0], k.shape[1], -1), 1)[..., 0, :] * k_k, HEADS)
    kk = kk / jnp.maximum(jnp.sqrt(jnp.sum(kk * kk, axis=-1, keepdims=True)), 1e-12)
    k = k * (1.0 + (a - 1.0) * _heads(k_a[None, None, :], HEADS))
    y = _rwkv7_scan(r, decay, k, v, kk, a)
    mean = jnp.mean(y, axis=-1, keepdims=True)
    var = jnp.mean(jnp.square(y - mean), axis=-1, keepdims=True)
    y = ((y - mean) * lax.rsqrt(var + RWKV_GN_EPS) * ln_w.reshape(HEADS, RWKV_HEAD)
         + ln_b.reshape(HEADS, RWKV_HEAD))
    y = y + jnp.sum(r * k * r_k, axis=-1, keepdims=True) * v
    return y.reshape(p.shape[0], p.shape[1], GROUP_WIDTH) * g


def _chunked_gla(q, k, v, log_g):
    b, t, h, dk = q.shape
    dv = v.shape[-1]
    n = t // CHUNK

    def to_chunks(z):
        return z.reshape(b, n, CHUNK, h, z.shape[-1]).transpose(1, 0, 3, 2, 4)

    causal = jnp.tril(jnp.ones((CHUNK, CHUNK), dtype=bool))[:, :, None]

    def step(S, inp):
        q_c, k_c, v_c, g_c = inp
        cum = jnp.cumsum(g_c, axis=-2)
        inter = jnp.einsum('bhtk,bhkv->bhtv', q_c * jnp.exp(cum), S)
        diff = cum[:, :, :, None, :] - cum[:, :, None, :, :]
        decay = jnp.exp(jnp.where(causal, diff, -jnp.inf))
        att = jnp.einsum('bhtk,bhsk,bhtsk->bhts', q_c, k_c, decay)
        intra = jnp.einsum('bhts,bhsv->bhtv', att, v_c)
        last = cum[:, :, -1:, :]
        S = (jnp.exp(last[:, :, 0, :])[..., None] * S
             + jnp.einsum('bhsk,bhsv->bhkv', k_c * jnp.exp(last - cum), v_c))
        return S, inter + intra

    S0 = jnp.zeros((b, h, dk, dv), jnp.float32)
    _, o = lax.scan(step, S0, (to_chunks(q), to_chunks(k), to_chunks(v), to_chunks(log_g)))
    return o.transpose(1, 0, 3, 2, 4).reshape(b, t, h, dv)


def _gla_group(p, gate_up, gate_b, norm_w):
    p = p.astype(jnp.float32)
    q, k, v, a_low, g = _split(p, (GLA_KEY_WIDTH, GLA_KEY_WIDTH, GROUP_WIDTH, GLA_GATE_RANK, GROUP_WIDTH))
    log_alpha = jax.nn.log_sigmoid(a_low @ gate_up + gate_b) / GLA_GATE_TAU
    o = _chunked_gla(_heads(q * GLA_HEAD_K ** -0.5, HEADS), _heads(k, HEADS),
                     _heads(v, HEADS), _heads(log_alpha, HEADS))
    o = _rms_norm(o, norm_w).reshape(p.shape[0], p.shape[1], GROUP_WIDTH)
    return o * jax.nn.silu(g)


def _lru_combine(left, right):
    a_l, b_l = left
    a_r, b_r = right
    return a_l * a_r, a_r * b_l + b_r


def _rglru_group(p, conv_w, conv_b, w_a, b_a, w_x, b_x, lam):
    p = p.astype(jnp.float32)
    xb, gate = _split(p, (GROUP_WIDTH, GROUP_WIDTH))
    xc = lax.conv_general_dilated(xb, conv_w.astype(jnp.float32)[:, None, :], window_strides=(1,),
                                  padding=[(LRU_CONV - 1, 0)],
                                  dimension_numbers=('NWC', 'WIO', 'NWC'),
                                  feature_group_count=GROUP_WIDTH) + conv_b
    xh = _heads(xc, HEADS)
    bsz, t = xc.shape[0], xc.shape[1]
    r = jax.nn.sigmoid(jnp.einsum('bthi,hij->bthj', xh, w_a).reshape(bsz, t, GROUP_WIDTH) + b_a)
    i = jax.nn.sigmoid(jnp.einsum('bthi,hij->bthj', xh, w_x).reshape(bsz, t, GROUP_WIDTH) + b_x)
    log_a = -LRU_C * r * jax.nn.softplus(-lam)
    a = jnp.exp(log_a)
    mult = jnp.sqrt(jnp.maximum(-jnp.expm1(2.0 * log_a), 0.0))
    _, h = lax.associative_scan(_lru_combine, (a, mult * (i * xc)), axis=1)
    return h * jax.nn.gelu(gate)


def _hgrn2_group(p, lb, norm_w):
    p = p.astype(jnp.float32)
    q, f_logit, i, g = _split(p, (GROUP_WIDTH,) * 4)
    lb = lb.astype(jnp.float32)
    f = lb + (1.0 - lb) * jax.nn.sigmoid(f_logit)
    o = _chunked_gla(_heads(q, HEADS), _heads(1.0 - f, HEADS), _heads(i, HEADS),
                     _heads(jnp.log(f), HEADS))
    o = _rms_norm(o, norm_w).reshape(p.shape[0], p.shape[1], GROUP_WIDTH)
    return o * jax.nn.silu(g)


def setup_inputs(seed: int = 0) -> dict:
    key = jax.random.key(seed)
    ks = iter(jax.random.split(key, 40))
    f32 = jnp.float32
    L = DEPTH

    def nrm(shape, scale):
        return jax.random.normal(next(ks), shape, f32) * scale

    def gain(shape):
        return 1.0 + 0.05 * jax.random.normal(next(ks), shape, f32)

    def unif(shape, lo, hi):
        return jax.random.uniform(next(ks), shape, f32, lo, hi)

    u = unif((L, GROUP_WIDTH), 0.9, 0.999) ** (1.0 / LRU_C)
    lru_lambda = jnp.log(u) - jnp.log1p(-u)
    return {
        'x': jax.random.normal(next(ks), (BATCH, SEQ, D_MODEL), f32),
        'norm_mix_pre': gain((L, D_MODEL)),
        'norm_mix_post': gain((L, D_MODEL)),
        'norm_ffn_pre': gain((L, D_MODEL)),
        'norm_ffn_post': gain((L, D_MODEL)),
        'w_in': nrm((L, D_MODEL, IN_COLS), D_MODEL ** -0.5),
        'w_out': nrm((L, N_GROUPS * GROUP_WIDTH, D_MODEL), (N_GROUPS * GROUP_WIDTH) ** -0.5),
        'rwkv_shift_mu': unif((L, RWKV_COLS), 0.0, 1.0),
        'rwkv_w0': unif((L, GROUP_WIDTH), -6.0, 0.0),
        'rwkv_w_up': nrm((L, RWKV_DECAY_RANK, GROUP_WIDTH), 0.1),
        'rwkv_a0': unif((L, GROUP_WIDTH), -1.0, 1.0),
        'rwkv_a_up': nrm((L, RWKV_A_RANK, GROUP_WIDTH), 0.1),
        'rwkv_g_up': nrm((L, RWKV_GATE_RANK, GROUP_WIDTH), RWKV_GATE_RANK ** -0.5),
        'rwkv_k_k': 0.85 + 0.05 * jax.random.normal(next(ks), (L, GROUP_WIDTH), f32),
        'rwkv_k_a': gain((L, GROUP_WIDTH)),
        'rwkv_r_k': nrm((L, HEADS, RWKV_HEAD), 0.1),
        'rwkv_ln_w': gain((L, GROUP_WIDTH)),
        'rwkv_ln_b': nrm((L, GROUP_WIDTH), 0.01),
        'gla_gate_up': nrm((L, GLA_GATE_RANK, GLA_KEY_WIDTH), GLA_GATE_RANK ** -0.5),
        'gla_gate_b': nrm((L, GLA_KEY_WIDTH), 0.1),
        'gla_norm_w': gain((L, GLA_HEAD_V)),
        'lru_conv_w': nrm((L, LRU_CONV, GROUP_WIDTH), LRU_CONV ** -0.5),
        'lru_conv_b': nrm((L, GROUP_WIDTH), 0.01),
        'lru_w_a': nrm((L, HEADS, LRU_BLOCK, LRU_BLOCK), LRU_BLOCK ** -0.5),
        'lru_b_a': nrm((L, GROUP_WIDTH), 0.01),
        'lru_w_x': nrm((L, HEADS, LRU_BLOCK, LRU_BLOCK), LRU_BLOCK ** -0.5),
        'lru_b_x': nrm((L, GROUP_WIDTH), 0.01),
        'lru_lambda': lru_lambda,
        'hgrn_lb_logits': nrm((L, GROUP_WIDTH), 1.0),
        'hgrn_norm_w': gain((L, HGRN_HEAD)),
        'ffn_w_gate_up': nrm((L, D_MODEL, 2 * FFN_HIDDEN), D_MODEL ** -0.5),
        'ffn_w_down': nrm((L, FFN_HIDDEN, D_MODEL), FFN_HIDDEN ** -0.5),
    }


def reference(x, norm_mix_pre, norm_mix_post, norm_ffn_pre, norm_ffn_post, w_in, w_out,
              rwkv_shift_mu, rwkv_w0, rwkv_w_up, rwkv_a0, rwkv_a_up, rwkv_g_up, rwkv_k_k,
              rwkv_k_a, rwkv_r_k, rwkv_ln_w, rwkv_ln_b, gla_gate_up, gla_gate_b, gla_norm_w,
              lru_conv_w, lru_conv_b, lru_w_a, lru_b_a, lru_w_x, lru_b_x, lru_lambda,
              hgrn_lb_logits, hgrn_norm_w, ffn_w_gate_up, ffn_w_down):
    lb_p = jax.nn.softmax(hgrn_lb_logits.astype(jnp.float32), axis=0)
    lower_bounds = jnp.cumsum(lb_p, axis=0) - lb_p[0:1]
    for l in range(DEPTH):
        h = _rms_norm(x, norm_mix_pre[l])
        proj = h @ w_in[l]
        p_a, p_b, p_c, p_d = _split(proj, (RWKV_COLS, GLA_COLS, LRU_COLS, HGRN_COLS))
        y_a = _rwkv7_group(p_a, rwkv_shift_mu[l], rwkv_w0[l], rwkv_w_up[l], rwkv_a0[l],
                           rwkv_a_up[l], rwkv_g_up[l], rwkv_k_k[l], rwkv_k_a[l], rwkv_r_k[l],
                           rwkv_ln_w[l], rwkv_ln_b[l])
        y_b = _gla_group(p_b, gla_gate_up[l], gla_gate_b[l], gla_norm_w[l])
        y_c = _rglru_group(p_c, lru_conv_w[l], lru_conv_b[l], lru_w_a[l], lru_b_a[l],
                           lru_w_x[l], lru_b_x[l], lru_lambda[l])
        y_d = _hgrn2_group(p_d, lower_bounds[l], hgrn_norm_w[l])
        mixed = jnp.concatenate([y_a, y_b, y_c, y_d], axis=-1).astype(x.dtype) @ w_out[l]
        x = x + _rms_norm(mixed, norm_mix_post[l])
        h = _rms_norm(x, norm_ffn_pre[l])
        gate, up = jnp.split(h @ ffn_w_gate_up[l], 2, axis=-1)
        x = x + _rms_norm((jax.nn.silu(gate) * up) @ ffn_w_down[l], norm_ffn_post[l])
    return x
```

```python
import contextlib
import numpy as np
import concourse.bass as bass
import concourse.mybir as mybir
from concourse.bass_utils import run_bass_kernel_spmd

F32 = mybir.dt.float32
BF16 = mybir.dt.bfloat16
AF = mybir.ActivationFunctionType
ALU = mybir.AluOpType

D = 1024
T = 2048
L = 4
SEG = 512
NSEG = T // SEG
NT = SEG // 128
FFN = 2816
NJ = FFN // 128
EPS = 1e-6
GN_EPS = 64e-5

RW_R, RW_K, RW_V, RW_LOW = 0, 256, 512, 768
GL_Q, GL_K, GL_V, GL_A, GL_G = 896, 1152, 1408, 1664, 1680
LR_X, LR_G = 1936, 2192
HG_Q, HG_F, HG_I, HG_G = 2448, 2704, 2960, 3216
WCOLS = 3472
NCHL = 8 + 2 * NJ + 24

QUEUES = ("pe", "act", "dve", "pool", "sp")


class Sched:
    def __init__(self, nc, same_engine_sync=True):
        self.nc = nc
        self.ops = []
        self.last_w = {}
        self.readers = {}
        self.same_engine_sync = same_engine_sync

    def add(self, eng, fn, reads=(), writes=(), dma=None, nosync=False):
        idx = len(self.ops)
        deps = set()
        if not nosync:
            for k in reads:
                w = self.last_w.get(k)
                if w is not None:
                    deps.add(w)
                if isinstance(k, tuple) and k[0] == "ps":
                    deps.update(r for r in self.readers.get(k, ()) if self.ops[r]["eng"] != eng)
            for k in writes:
                w = self.last_w.get(k)
                if w is not None:
                    deps.add(w)
                deps.update(self.readers.get(k, ()))
        self.ops.append(dict(eng=eng, fn=fn, deps=deps, dma=dma))
        for k in reads:
            lst = self.readers.setdefault(k, [])
            if dma is None:
                lst[:] = [r for r in lst if not (self.ops[r]["dma"] is None and self.ops[r]["eng"] == eng)]
            lst.append(idx)
        for k in writes:
            self.last_w[k] = idx
            self.readers[k] = []
        return idx

    def emit(self, final_wait_eng="sp"):
        nc = self.nc
        ops = self.ops
        needed = set()
        for o in ops:
            needed |= o["deps"]
        last = {}
        for i, o in enumerate(ops):
            if o["dma"] is None:
                last[o["eng"]] = i
            else:
                last[("dma", o["dma"])] = i
        needed |= set(last.values())
        val, cnt, semkey_of = {}, {}, {}
        for i, o in enumerate(ops):
            if o["dma"] is not None:
                sk = ("dma", o["dma"])
                cnt[sk] = cnt.get(sk, 0) + 16
            else:
                sk = ("eng", o["eng"])
                if i in needed:
                    cnt[sk] = cnt.get(sk, 0) + 1
            val[i] = cnt.get(sk, 0)
            semkey_of[i] = sk
        totals = dict(cnt)
        stack = contextlib.ExitStack()
        sems = {}
        for n, sk in enumerate(totals):
            sems[sk] = stack.enter_context(nc.semaphore("s%d" % n))
        self.n_sems = len(sems)
        per_eng = {q: [] for q in QUEUES}
        for i, o in enumerate(ops):
            per_eng[o["eng"]].append(i)

        def wait_list(i, known):
            o = ops[i]
            need = {}
            for d in o["deps"]:
                od = ops[d]
                sk = semkey_of[d]
                if od["dma"] is None and o["dma"] is None and od["eng"] == o["eng"]:
                    if od["eng"] == "pe" or not self.same_engine_sync:
                        continue
                v = val[d]
                if v > need.get(sk, 0):
                    need[sk] = v
            out = []
            for sk, v in need.items():
                if known.get(sk, 0) >= v:
                    continue
                known[sk] = v
                out.append((sk, v))
            return out

        block = stack.enter_context(nc.Block())
        handles = dict(pe="tensor", act="scalar", dve="vector", pool="gpsimd", sp="sync")
        self.n_waits = 0

        def make_body(q):
            def body(e):
                known = {}
                for i in per_eng[q]:
                    o = ops[i]
                    for sk, v in wait_list(i, known):
                        e.wait_ge(sems[sk], v)
                        self.n_waits += 1
                    ins = o["fn"](e)
                    if o["dma"] is not None:
                        ins.then_inc(sems[semkey_of[i]], 16)
                    elif i in needed:
                        ins.then_inc(sems[semkey_of[i]], 1)
                if q == final_wait_eng:
                    for sk, v in totals.items():
                        if known.get(sk, 0) < v:
                            e.wait_ge(sems[sk], v)
            return body

        for q in QUEUES:
            if per_eng[q] or q == final_wait_eng:
                getattr(block, handles[q])(make_body(q))
        stack.close()


class Tl:
    __slots__ = ("ap", "key")

    def __init__(self, ap, key):
        self.ap = ap
        self.key = key

    def __getitem__(self, idx):
        return Tl(self.ap[idx], self.key)

    def re(self, s, **kw):
        return Tl(self.ap.rearrange(s, **kw), self.key)


def _ap(x):
    return x.ap if isinstance(x, Tl) else x


def _keys(*xs):
    return [x.key for x in xs if isinstance(x, Tl)]


class Pool:
    def __init__(self, tiles):
        self.free = list(tiles)

    def get(self):
        return self.free.pop(0)

    def put(self, *ts):
        for t in ts:
            self.free.append(t)


class KB:
    def __init__(self, nc, depth, dbg):
        self.nc = nc
        self.depth = depth
        self.dbg = dbg or ()
        self.S = Sched(nc)
        self.dumps = {}
        self.marks = []

    def act(self, out, in_, func, scale=1.0, bias=0.0):
        kw = {}
        self.S.add("act", lambda e: e.activation(out=_ap(out), in_=_ap(in_), func=func, scale=_ap(scale), bias=_ap(bias)),
                   reads=_keys(in_, scale, bias), writes=_keys(out))

    def tt(self, out, a, b, op):
        self.S.add("dve", lambda e: e.tensor_tensor(out=_ap(out), in0=_ap(a), in1=_ap(b), op=op),
                   reads=_keys(a, b), writes=_keys(out))

    def ts(self, out, a, s1, s2, op0, op1=None):
        if op1 is None:
            self.S.add("dve", lambda e: e.tensor_scalar(out=_ap(out), in0=_ap(a), scalar1=_ap(s1), scalar2=None, op0=op0),
                       reads=_keys(a, s1), writes=_keys(out))
        else:
            self.S.add("dve", lambda e: e.tensor_scalar(out=_ap(out), in0=_ap(a), scalar1=_ap(s1), scalar2=_ap(s2), op0=op0, op1=op1),
                       reads=_keys(a, s1, s2), writes=_keys(out))

    def stt(self, out, a, s, b, op0, op1):
        self.S.add("dve", lambda e: e.scalar_tensor_tensor(out=_ap(out), in0=_ap(a), scalar=_ap(s), in1=_ap(b), op0=op0, op1=op1),
                   reads=_keys(a, s, b), writes=_keys(out))

    def scan(self, out, d0, d1, init):
        self.S.add("dve", lambda e: e.tensor_tensor_scan(out=_ap(out), data0=_ap(d0), data1=_ap(d1), initial=_ap(init), op0=ALU.mult, op1=ALU.add),
                   reads=_keys(d0, d1, init), writes=_keys(out))

    def rsqrt(self, out, in_, scale=1.0, bias=0.0):
        self.act(out, in_, AF.Ln, scale=scale, bias=bias)
        self.act(out, out, AF.Exp, scale=-0.5)

    def recip(self, out, in_):
        self.S.add("dve", lambda e: e.reciprocal(out=_ap(out), in_=_ap(in_)), reads=_keys(in_), writes=_keys(out))

    def vcopy(self, out, in_):
        self.S.add("dve", lambda e: e.tensor_copy(out=_ap(out), in_=_ap(in_)), reads=_keys(in_), writes=_keys(out))

    def acopy(self, out, in_):
        self.act(out, in_, AF.Copy)

    def memset(self, out, v, eng="dve"):
        self.S.add(eng, lambda e: e.memset(_ap(out), v), writes=_keys(out))

    def mm(self, out, lhsT, rhs, start=True, stop=True):
        self.S.add("pe", lambda e: e.matmul(_ap(out), lhsT=_ap(lhsT), rhs=_ap(rhs), start=start, stop=stop),
                   reads=_keys(lhsT, rhs), writes=_keys(out))

    def tr(self, out, in_, ident):
        self.S.add("pe", lambda e: e.transpose(out=_ap(out), in_=_ap(in_), identity=_ap(ident)),
                   reads=_keys(in_, ident), writes=_keys(out))

    def dma(self, q, out, in_, semkey, nosync=False):
        self.S.add(q, lambda e: e.dma_start(out=_ap(out), in_=_ap(in_)), reads=_keys(in_), writes=_keys(out),
                   dma=semkey, nosync=nosync)

    def mark(self, name):
        self.marks.append((name, sum(1 for o in self.S.ops if o['eng'] == 'pe'), sum(1 for o in self.S.ops if o['eng'] == 'dve')))

    def dump(self, name, t, shape):
        if name not in self.dbg:
            return
        d = self.nc.dram_tensor("dbg_" + name, list(shape), F32, kind="ExternalOutput").ap()
        self.dumps[name] = d
        return d

    def dump_tile(self, name, t, rows, c0, ncols, total_cols, r0=0, total_rows=None):
        if name not in self.dbg:
            return
        if name not in self.dumps:
            self.dumps[name] = self.nc.dram_tensor("dbg_" + name, [total_rows or rows, total_cols], F32, kind="ExternalOutput").ap()
        d = self.dumps[name]
        if t.ap.dtype != F32:
            tmp = self.f32.get()
            self.vcopy(tmp[0:rows, 0:ncols], t)
            self.dma("sp", d[r0:r0 + rows, c0:c0 + ncols], tmp[0:rows, 0:ncols], "out")
            self.f32.put(tmp)
        else:
            self.dma("sp", d[r0:r0 + rows, c0:c0 + ncols], t, "out")


PCOL = {}


def _pcol_layout():
    names = []

    def add(n, k):
        PCOL[n] = (sum(x[1] for x in names), k)
        names.append((n, k))
    add("g_mix_pre", 8); add("g_mix_post", 8); add("g_ffn_pre", 8); add("g_ffn_post", 8)
    add("mu", 7); add("w0", 2); add("a0", 2); add("k_k", 2); add("k_a", 2); add("r_k", 2); add("ln_w", 2); add("ln_b", 2)
    add("gla_b", 2); add("gla_nw", 1)
    add("conv_w", 8); add("conv_b", 2); add("b_a", 2); add("b_x", 2); add("lam", 2)
    add("lb_logits", 8); add("hg_nw", 1)
    add("omu", 7); add("nw0", 2); add("omka", 2); add("ngla_b", 2); add("lc", 2); add("lc2", 2); add("lb", 2); add("omlb", 2)
    add("tmp", 8)
    return sum(x[1] for x in names)


NPCOL = _pcol_layout()
NPCOL_IN = PCOL["omu"][0]


def _host_layout(inp, l):
    f = np.float32
    w_in = np.asarray(inp["w_in"][l], f)
    arr = np.zeros((D, WCOLS), f)
    arr[:, 0:896] = w_in[:, 0:896]
    g0 = 896
    for nm, dst in (("q", GL_Q), ("k", GL_K)):
        src = g0 + (0 if nm == "q" else 128)
        for h in range(4):
            arr[:, dst + h * 64: dst + h * 64 + 32] = w_in[:, src + h * 32: src + (h + 1) * 32]
    arr[:, GL_V:GL_V + 256] = w_in[:, g0 + 256: g0 + 512]
    arr[:, GL_A:GL_A + 16] = w_in[:, g0 + 512: g0 + 528]
    arr[:, GL_G:GL_G + 256] = w_in[:, g0 + 528: g0 + 784]
    arr[:, LR_X:LR_X + 512] = w_in[:, 1680:2192]
    arr[:, HG_Q:HG_Q + 1024] = w_in[:, 2192:3216]
    win_h = arr.reshape(8, 128, WCOLS)

    ch = np.zeros((NCHL, 128, 1024), f)
    w_out = np.asarray(inp["w_out"][l], f)
    for fo in range(8):
        ch[fo] = w_out[:, fo * 128:(fo + 1) * 128].reshape(8, 128, 128).transpose(1, 0, 2).reshape(128, 1024)
    wgu = np.asarray(inp["ffn_w_gate_up"][l], f)
    for j in range(NJ):
        ch[8 + 2 * j] = wgu[:, j * 128:(j + 1) * 128].reshape(8, 128, 128).transpose(1, 0, 2).reshape(128, 1024)
        ch[8 + 2 * j + 1] = wgu[:, FFN + j * 128: FFN + (j + 1) * 128].reshape(8, 128, 128).transpose(1, 0, 2).reshape(128, 1024)
    wd = np.asarray(inp["ffn_w_down"][l], f)
    for fo in range(8):
        blk = wd[:, fo * 128:(fo + 1) * 128].reshape(NJ, 128, 128).transpose(1, 0, 2)
        for pc, (j0, j1) in enumerate(((0, 8), (8, 16), (16, 22))):
            ch[8 + 2 * NJ + fo * 3 + pc][:, 0:(j1 - j0) * 128] = blk[:, j0:j1, :].reshape(128, (j1 - j0) * 128)

    pc = np.zeros((128, NPCOL_IN), f)

    def put(nm, vec):
        vec = np.asarray(vec, f).reshape(-1)
        c0, k = PCOL[nm]
        assert vec.size == 128 * k, (nm, vec.size, k)
        pc[:, c0:c0 + k] = vec.reshape(k, 128).T
    put("g_mix_pre", inp["norm_mix_pre"][l]); put("g_mix_post", inp["norm_mix_post"][l])
    put("g_ffn_pre", inp["norm_ffn_pre"][l]); put("g_ffn_post", inp["norm_ffn_post"][l])
    put("mu", inp["rwkv_shift_mu"][l])
    for nm, key in (("w0", "rwkv_w0"), ("a0", "rwkv_a0"), ("k_k", "rwkv_k_k"), ("k_a", "rwkv_k_a"), ("r_k", "rwkv_r_k"),
                    ("ln_w", "rwkv_ln_w"), ("ln_b", "rwkv_ln_b"), ("conv_b", "lru_conv_b"), ("b_a", "lru_b_a"),
                    ("b_x", "lru_b_x"), ("lam", "lru_lambda")):
        put(nm, inp[key][l])
    gb = np.zeros(256, f)
    gbs = np.asarray(inp["gla_gate_b"][l], f)
    for h in range(4):
        gb[h * 64:h * 64 + 32] = gbs[h * 32:(h + 1) * 32]
    put("gla_b", gb)
    put("gla_nw", np.tile(np.asarray(inp["gla_norm_w"][l], f), 2))
    put("hg_nw", np.tile(np.asarray(inp["hgrn_norm_w"][l], f), 2))
    cw = np.asarray(inp["lru_conv_w"][l], f)
    put("conv_w", np.stack([cw[:, 0:128], cw[:, 128:256]], 0).reshape(-1))
    lbl = np.asarray(inp["hgrn_lb_logits"], f)
    put("lb_logits", np.stack([lbl[:, 0:128], lbl[:, 128:256]], 0).reshape(-1))

    pm = np.zeros((128, 1024), f)
    pm[0:32, 0:256] = inp["rwkv_w_up"][l]
    pm[32:64, 0:256] = inp["rwkv_a_up"][l]
    pm[64:128, 0:256] = inp["rwkv_g_up"][l]
    gu = np.asarray(inp["gla_gate_up"][l], f)
    for h in range(4):
        pm[0:16, 256 + h * 64: 256 + h * 64 + 32] = gu[:, h * 32:(h + 1) * 32]
    wa = np.asarray(inp["lru_w_a"][l], f)
    wx = np.asarray(inp["lru_w_x"][l], f)
    for r in range(2):
        for hh in range(2):
            pm[hh * 64:(hh + 1) * 64, 512 + r * 128 + hh * 64: 512 + r * 128 + (hh + 1) * 64] = wa[2 * r + hh]
            pm[hh * 64:(hh + 1) * 64, 768 + r * 128 + hh * 64: 768 + r * 128 + (hh + 1) * 64] = wx[2 * r + hh]
    return win_h, ch, pc, pm


NCONST = 128 * 6 + 4 + 512 * 2


def _host_consts():
    c = np.zeros((128, NCONST), np.float32)
    i = np.arange(128)
    c[:, 0:128] = np.eye(128)
    c[:, 128:256] = (i[:, None] // 64 == i[None, :] // 64)
    same64 = (i[:, None] // 64 == i[None, :] // 64)
    c[:, 256:384] = same64 & (i[:, None] < i[None, :])
    c[:, 384:512] = same64 & (i[:, None] > i[None, :])
    c[:, 512:640] = same64 & (i[:, None] <= i[None, :])
    same32 = (i[:, None] // 32 == i[None, :] // 32)
    c[:, 640:768] = same32 & (i[:, None] <= i[None, :])
    for n in range(4):
        c[:, 768 + n] = (i // 32 == n)
    t = np.arange(512)
    c[:, 772:772 + 512] = (t % 32 != 0)[None, :]
    c[:, 772 + 512:772 + 1024] = (t % 64 != 0)[None, :]
    return c


def build(depth=L, dbg=None, dbg_stop=None):
    nc = bass.Bass("TRN2", target_bir_lowering=False)
    kb = KB(nc, depth, dbg)
    S = kb.S
    x_d = nc.dram_tensor("xT", [D, T], F32, kind="ExternalInput").ap()
    win_d = nc.dram_tensor("win", [depth, 8, 128, WCOLS], F32, kind="ExternalInput").ap()
    wch_d = nc.dram_tensor("wch", [depth, NCHL, 128, 1024], F32, kind="ExternalInput").ap()
    pcol_d = nc.dram_tensor("pcol", [depth, 128, NPCOL_IN], F32, kind="ExternalInput").ap()
    pmat_d = nc.dram_tensor("pmat", [depth, 128, 1024], F32, kind="ExternalInput").ap()
    const_d = nc.dram_tensor("consts", [128, NCONST], F32, kind="ExternalInput").ap()
    out_d = nc.dram_tensor("outT", [D, T], F32, kind="ExternalOutput").ap()

    st = contextlib.ExitStack()

    def sb(name, shape, dt):
        return st.enter_context(nc.sbuf_tensor(name, shape, dt))

    xT_t = sb("xT_sb", [128, 8, T], F32)
    win_t = sb("win_sb", [128, 8, WCOLS], BF16)
    pcol_t = sb("pcol_sb", [128, NPCOL], F32)
    pmat_t = sb("pmat_sb", [128, 1024], F32)
    cst_t = sb("cst_sb", [128, NCONST], F32)
    cstb_t = sb("cstb_sb", [128, 768], BF16)
    NA = 6
    wA_t = sb("wA_sb", [128, NA, 1024], BF16)
    NF32, NB16 = 12, 36
    f32_t = sb("f32pool", [128, NF32, 512], F32)
    b16_t = sb("b16pool", [128, NB16, 512], BF16)
    carry_t = sb("carry_sb", [128, 7 + 2 + 2 * 3], F32)
    st_t = sb("state_sb", [128, 6, 64], F32)
    ttbd_t = sb("ttbd_sb", [128, 4, 128], F32)
    small_t = sb("small_sb", [128, 64], F32)
    psf = [st.enter_context(nc.psum_tensor("ps%d" % i, [128, 512], F32)) for i in range(8)]

    xT = [[Tl(xT_t[:, dt, s * SEG:(s + 1) * SEG], ("x", dt, s)) for s in range(NSEG)] for dt in range(8)]
    win = [Tl(win_t[:, dt, :], ("win", dt)) for dt in range(8)]
    pcol = Tl(pcol_t[:], "pcol")
    pmat = Tl(pmat_t[:], "pmat")
    cst = Tl(cst_t[:], "cst")
    cstb = Tl(cstb_t[:], "cstb")
    kb.f32 = Pool([Tl(f32_t[:, i, :], ("f32", i)) for i in range(NF32)])
    kb.b16 = Pool([Tl(b16_t[:, i, :], ("b16", i)) for i in range(NB16)])
    kb.ps = Pool([Tl(psf[i][:], ("ps", i)) for i in range(8)])
    carry = Tl(carry_t[:], "carry")
    small = Tl(small_t[:], "small")
    f32, b16, ps = kb.f32, kb.b16, kb.ps

    ident = cst[:, 0:128]
    blk2 = cst[:, 128:256]
    mask4 = cst[:, 768:772]
    cmask32 = cst[:, 772:772 + 512]
    cmask64 = cst[:, 772 + 512:772 + 1024]
    identb = cstb[:, 0:128]
    onesb = cstb[:, 128:256]
    m_su, m_sl, m_iu, m_iu32 = cstb[:, 256:384], cstb[:, 384:512], cstb[:, 512:640], cstb[:, 640:768]

    def col(name, j=0):
        c0, k = PCOL[name]
        return pcol[:, c0 + j:c0 + j + 1]

    kb.dma("sp", cst, const_d[:, :], "cst")
    kb.dma("pool", cstb, const_d[:, 0:768], "cstb")
    for s in range(NSEG):
        S.add("sp", (lambda s: lambda e: e.dma_start(out=xT_t[:, :, s * SEG:(s + 1) * SEG],
                                                      in_=x_d[:, s * SEG:(s + 1) * SEG].rearrange("(dt p) t -> p dt t", p=128)))(s),
              writes=[("x", dt, s) for dt in range(8)], dma=("xin", s))
    allones = Tl(sb("allones", [128, 128], BF16)[:], "allones")
    kb.memset(allones, 1.0)

    wq = []
    for l in range(depth):
        for s in range(NSEG):
            for c in range(NCHL):
                wq.append((l, c))
    wstate = dict(issued=0, used=0)

    def next_chunk():
        i = wstate["used"]
        while wstate["issued"] < min(len(wq), i + NA - 1):
            j = wstate["issued"]
            lj, cj = wq[j]
            slot = j % NA
            kb.dma("pool", Tl(wA_t[:, slot, :], ("wA", slot)), wch_d[lj, cj], ("wA", slot))
            wstate["issued"] += 1
        wstate["used"] += 1
        return Tl(wA_t[:, i % NA, :], ("wA", i % NA))

    def norm_stats_rstd(sq_list_fn, n, inv_n, eps, lhs):
        ssp = ps.get()
        for i in range(n):
            sq = sq_list_fn(i)
            kb.mm(ssp, lhs, sq, start=(i == 0), stop=(i == n - 1))
            b16.put(sq)
        rstd = f32.get()
        kb.rsqrt(rstd, ssp, scale=inv_n, bias=eps)
        ps.put(ssp)
        return rstd

    def rms_in(s, gname):
        def sqf(dt):
            sq = b16.get()
            kb.act(sq, xT[dt][s], AF.Square)
            return sq
        rstd = norm_stats_rstd(sqf, 8, 1.0 / D, EPS, allones)
        hs = []
        for dt in range(8):
            h = b16.get()
            kb.stt(h, xT[dt][s], col(gname, dt), rstd, ALU.mult, ALU.mult)
            hs.append(h)
        f32.put(rstd)
        return hs

    def proj(hT, c0, m):
        p = ps.get()
        for dt in range(8):
            kb.mm(p[0:m, :], win[dt][:, c0:c0 + m], hT[dt], start=(dt == 0), stop=(dt == 7))
        return p

    def proj_tok(hT, c0, n, tt, p):
        for dt in range(8):
            kb.mm(p, hT[dt][:, tt * 128:(tt + 1) * 128], win[dt][:, c0:c0 + n], start=(dt == 0), stop=(dt == 7))

    def post_norm_residual(s, mtiles, sqs, gname):
        it = iter(sqs)
        rstd = norm_stats_rstd(lambda i: next(it), 8, 1.0 / D, EPS, allones)
        for fo in range(8):
            kb.stt(mtiles[fo], mtiles[fo], col(gname, fo), rstd, ALU.mult, ALU.mult)
            kb.tt(xT[fo][s], xT[fo][s], mtiles[fo], ALU.add)
            f32.put(mtiles[fo])
        f32.put(rstd)

    def gla_core(hT, r, q_ps, k_sb, lg, vcol, gcol, nwcol, qscale, Scar, yout, tag, l, s):
        cum = f32.get()
        kb.scan(cum, cmask32, lg, 0.0)
        f32.put(lg)
        e = f32.get()
        kb.act(e, cum, AF.Exp)
        qe = b16.get()
        kb.stt(qe, q_ps, qscale, e, ALU.mult, ALU.mult)
        ps.put(q_ps)
        kb.act(e, cum, AF.Exp, scale=-1.0)
        ke = b16.get()
        kb.tt(ke, k_sb, e, ALU.mult)
        cl = cum[:, 31::32]
        an = small[:, r * 16:(r + 1) * 16]
        kb.act(an, cl, AF.Exp)
        kb.tt(e.re("p (n c) -> p n c", c=32), Tl(cl.ap.unsqueeze(2).broadcast_to([128, 16, 32]), cum.key),
              cum.re("p (n c) -> p n c", c=32), ALU.subtract)
        kb.act(e, e, AF.Exp)
        kl = f32.get()
        kb.tt(kl, k_sb, e, ALU.mult)
        f32.put(e, cum, k_sb)
        o_sb = f32.get()
        kb.mark('%s%d.p1' % (tag, r))
        Gs = [f32.get(), f32.get()]
        atts = [b16.get(), b16.get()]
        vtoks = b16.get()
        klTs = b16.get()
        vms = [b16.get() for _ in range(NT)]
        TS = [slice(tt * 128, (tt + 1) * 128) for tt in range(NT)]
        for tt in range(NT):
            apb = [ps.get(), ps.get()]
            att = atts[tt // 2][:, (tt % 2) * 256:(tt % 2) * 256 + 256]
            for hh in range(2):
                hs_ = slice(hh * 64, (hh + 1) * 64)
                kb.mm(apb[hh][:, 0:128], ke[hs_, TS[tt]], qe[hs_, TS[tt]])
                kb.tt(att[:, hh * 128:(hh + 1) * 128], apb[hh][:, 0:128], m_iu32, ALU.mult)
            ps.put(*apb)
        for tt in range(NT):
            tp = ps.get()
            kb.mm(tp[:, 0:128], kl[:, TS[tt]], ident)
            kb.acopy(klTs[:, TS[tt]], tp[:, 0:128])
            ps.put(tp)
            vp = ps.get()
            proj_tok(hT, vcol, 128, tt, vp[:, 0:128])
            kb.tt(vms[tt].re("p (n v) -> p n v", n=4), Tl(vp.ap[:, 0:128].unsqueeze(1).broadcast_to([128, 4, 128]), vp.key),
                  Tl(mask4.ap.unsqueeze(2).broadcast_to([128, 4, 128]), mask4.key), ALU.mult)
            kb.vcopy(vtoks[:, TS[tt]], vp[:, 0:128])
            ps.put(vp)
        for tt in range(NT):
            gp = ps.get()
            for hh in range(2):
                hs_ = slice(hh * 64, (hh + 1) * 64)
                for n in range(4):
                    kb.mm(gp[hs_, n * 64:(n + 1) * 64], klTs[:, tt * 128 + hh * 64: tt * 128 + (hh + 1) * 64],
                          vms[tt][:, n * 128 + hh * 64: n * 128 + (hh + 1) * 64])
            kb.acopy(Gs[tt // 2][:, (tt % 2) * 256:(tt % 2) * 256 + 256], gp[:, 0:256])
            ps.put(gp)
        for tt in range(NT):
            att = atts[tt // 2][:, (tt % 2) * 256:(tt % 2) * 256 + 256]
            opb = [ps.get(), ps.get()]
            for hh in range(2):
                hs_ = slice(hh * 64, (hh + 1) * 64)
                kb.mm(opb[hh][hs_, 0:128], vtoks[:, tt * 128 + hh * 64: tt * 128 + (hh + 1) * 64], att[:, hh * 128:(hh + 1) * 128])
                kb.acopy(o_sb[hs_, TS[tt]], opb[hh][hs_, 0:128])
            ps.put(*opb)
        b16.put(klTs, *vms)
        f32.put(kl)
        b16.put(ke, vtoks, *atts)
        kb.mark('%s%d.p2' % (tag, r))
        Sch = [f32.get(), f32.get()]
        kb.vcopy(Sch[0][:, 0:64], Scar)
        for n in range(16):
            src = Sch[n // 8][:, (n % 8) * 64:(n % 8) * 64 + 64]
            dst = Sch[(n + 1) // 8][:, ((n + 1) % 8) * 64:((n + 1) % 8) * 64 + 64] if n < 15 else Scar
            kb.stt(dst, src, an[:, n:n + 1], Gs[n // 8][:, (n % 8) * 64:(n % 8) * 64 + 64], ALU.mult, ALU.add)
        sbf = [b16.get(), b16.get()]
        kb.acopy(sbf[0], Sch[0])
        kb.acopy(sbf[1], Sch[1])
        f32.put(*Gs)
        f32.put(*Sch)
        kb.mark('%s%d.p3' % (tag, r))
        for tt in range(NT):
            tsl = slice(tt * 128, (tt + 1) * 128)
            opb = [ps.get(), ps.get()]
            for hh in range(2):
                hs_ = slice(hh * 64, (hh + 1) * 64)
                for n in range(4):
                    c = tt * 4 + n
                    kb.mm(opb[hh][hs_, n * 32:(n + 1) * 32], sbf[c // 8][hs_, (c % 8) * 64:(c % 8) * 64 + 64],
                          qe[hs_, tt * 128 + n * 32: tt * 128 + (n + 1) * 32])
                kb.tt(o_sb[hs_, tsl], opb[hh][hs_, 0:128], o_sb[hs_, tsl], ALU.add)
            ps.put(*opb)
        b16.put(qe, *sbf)
        kb.dump_tile(tag + "_o", o_sb, 128, s * SEG, SEG, T, r0=r * 128, total_rows=256) if l == 0 else None
        kb.mark('%s%d.fin' % (tag, r))
        osq = f32.get()
        kb.act(osq, o_sb, AF.Square)
        ssp = ps.get()
        kb.mm(ssp, blk2, osq)
        kb.rsqrt(osq, ssp, scale=1.0 / 64, bias=EPS)
        ps.put(ssp)
        kb.stt(o_sb, o_sb, nwcol, osq, ALU.mult, ALU.mult)
        gp = proj(hT, gcol, 128)
        kb.act(osq, gp, AF.Silu)
        ps.put(gp)
        kb.tt(yout, o_sb, osq, ALU.mult)
        f32.put(osq, o_sb)

    def mixer_hgrn(l, s, hT, ycat):
        for r in range(2):
            q_ps = proj(hT, HG_Q + r * 128, 128)
            fp = proj(hT, HG_F + r * 128, 128)
            f = f32.get()
            kb.act(f, fp, AF.Sigmoid)
            ps.put(fp)
            kb.ts(f, f, col("omlb", r), col("lb", r), ALU.mult, ALU.add)
            k = f32.get()
            kb.ts(k, f, -1.0, 1.0, ALU.mult, ALU.add)
            kb.act(f, f, AF.Ln)
            gla_core(hT, r, q_ps, k, f, HG_I + r * 128, HG_G + r * 128, col("hg_nw"), 1.0,
                     Tl(st_t[:, 2 + r, :], ("st", 2 + r)), ycat[6 + r], "hg", l, s)

    def mixer_gla(l, s, hT, ycat):
        alp = proj(hT, GL_A, 16)
        alow = f32.get()
        kb.acopy(alow[0:16, :], alp[0:16, :])
        ps.put(alp)
        for r in range(2):
            q_ps = proj(hT, GL_Q + r * 128, 128)
            kp = proj(hT, GL_K + r * 128, 128)
            k = f32.get()
            kb.acopy(k, kp)
            ps.put(kp)
            mp = ps.get()
            kb.mm(mp, pmat[0:16, 256 + r * 128: 256 + (r + 1) * 128], alow[0:16, :])
            lg = f32.get()
            kb.act(lg, mp, AF.Softplus, scale=-1.0, bias=col("ngla_b", r))
            ps.put(mp)
            kb.ts(lg, lg, -1.0 / 16.0, None, ALU.mult)
            gla_core(hT, r, q_ps, k, lg, GL_V + r * 128, GL_G + r * 128, col("gla_nw"), 32.0 ** -0.5,
                     Tl(st_t[:, r, :], ("st", r)), ycat[2 + r], "gl", l, s)
        f32.put(alow)

    def lru_pair(l, s, hT, ycat, r):
        if True:
            xp = proj(hT, LR_X + r * 128, 128)
            tail = carry[:, 9 + 3 * r: 12 + 3 * r]
            cw = lambda kk_: col("conv_w", r * 4 + kk_)
            xc = f32.get()
            kb.ts(xc, xp, cw(3), col("conv_b", r), ALU.mult, ALU.add)
            for kk_ in range(3):
                sh = 3 - kk_
                kb.stt(xc[:, sh:SEG], xp[:, 0:SEG - sh], cw(kk_), xc[:, sh:SEG], ALU.mult, ALU.add)
                kb.stt(xc[:, 0:sh], tail[:, 3 - sh:3], cw(kk_), xc[:, 0:sh], ALU.mult, ALU.add)
            kb.vcopy(tail, xp[:, SEG - 3:SEG])
            ps.put(xp)
            yield
            gap = ps.get()
            kb.mm(gap, pmat[:, 512 + r * 128: 512 + (r + 1) * 128], xc)
            rg = f32.get()
            kb.act(rg, gap, AF.Sigmoid, bias=col("b_a", r))
            ps.put(gap)
            gxp = ps.get()
            kb.mm(gxp, pmat[:, 768 + r * 128: 768 + (r + 1) * 128], xc)
            ig = f32.get()
            kb.act(ig, gxp, AF.Sigmoid, bias=col("b_x", r))
            ps.put(gxp)
            yield
            a = f32.get()
            kb.act(a, rg, AF.Exp, scale=col("lc", r))
            kb.act(rg, rg, AF.Exp, scale=col("lc2", r))
            kb.act(rg, rg, AF.Sqrt, scale=-1.0, bias=1.0)
            kb.tt(ig, ig, xc, ALU.mult)
            kb.tt(ig, ig, rg, ALU.mult)
            yield
            hc = carry[:, 7 + r: 8 + r]
            kb.scan(xc, a, ig, hc)
            kb.vcopy(hc, xc[:, SEG - 1:SEG])
            yield
            gp = proj(hT, LR_G + r * 128, 128)
            kb.act(rg, gp, AF.Gelu_apprx_tanh)
            ps.put(gp)
            if ycat[4 + r] is None:
                ycat[4 + r] = b16.get()
            kb.tt(ycat[4 + r], xc, rg, ALU.mult)
            f32.put(xc, rg, ig, a)

    def mixer_lru(l, s, hT, ycat):
        gens = [lru_pair(l, s, hT, ycat, 0), lru_pair(l, s, hT, ycat, 1)]
        while gens:
            for g in list(gens):
                try:
                    next(g)
                except StopIteration:
                    gens.remove(g)

    def shift(p_ps, mu_i, out):
        cc = carry[:, mu_i:mu_i + 1]
        kb.ts(out, p_ps, col("omu", mu_i), None, ALU.mult)
        kb.stt(out[:, 1:SEG], p_ps[:, 0:SEG - 1], col("mu", mu_i), out[:, 1:SEG], ALU.mult, ALU.add)
        kb.stt(out[:, 0:1], cc, col("mu", mu_i), out[:, 0:1], ALU.mult, ALU.add)
        kb.vcopy(cc, p_ps[:, SEG - 1:SEG])
        ps.put(p_ps)

    def bc2(t):
        return Tl(t.ap.unsqueeze(1).broadcast_to([128, 2, 128]), t.key)

    def bc4(t):
        return Tl(t.ap.unsqueeze(1).broadcast_to([128, 4, 128]), t.key)

    def mixer_rwkv(l, s, hT, ycat):
        Lw = f32.get()
        shift(proj(hT, RW_LOW, 128), 6, Lw)
        kb.act(Lw[0:32, :], Lw[0:32, :], AF.Tanh)
        kb.act(Lw[64:128, :], Lw[64:128, :], AF.Sigmoid)
        for r in range(2):
            rs = slice(r * 128, (r + 1) * 128)
            ru, ku, vu = f32.get(), f32.get(), f32.get()
            shift(proj(hT, RW_R + r * 128, 128), 0 + r, ru)
            shift(proj(hT, RW_K + r * 128, 128), 2 + r, ku)
            shift(proj(hT, RW_V + r * 128, 128), 4 + r, vu)
            wp = ps.get()
            kb.mm(wp, pmat[0:32, rs], Lw[0:32, :])
            e1 = f32.get()
            kb.act(e1, wp, AF.Softplus, scale=-1.0, bias=col("nw0", r))
            ps.put(wp)
            kb.act(e1, e1, AF.Exp, scale=-1.0, bias=-0.5)
            cpos = f32.get()
            kb.scan(cpos, cmask64, e1, 0.0)
            ap_ = ps.get()
            kb.mm(ap_, pmat[32:64, rs], Lw[32:64, :])
            al = f32.get()
            kb.act(al, ap_, AF.Sigmoid, bias=col("a0", r))
            ps.put(ap_)
            kk = f32.get()
            kb.ts(kk, ku, col("k_k", r), None, ALU.mult)
            t8 = f32.get()
            kb.act(t8, kk, AF.Square)
            sp_ = ps.get()
            kb.mm(sp_, blk2, t8)
            kb.rsqrt(t8, sp_, bias=1e-16)
            ps.put(sp_)
            kb.tt(kk, kk, t8, ALU.mult)
            kb.ts(t8, al, col("k_a", r), col("omka", r), ALU.mult, ALU.add)
            kb.tt(ku, ku, t8, ALU.mult)
            kb.tt(t8, ru, ku, ALU.mult)
            kb.ts(t8, t8, col("r_k", r), None, ALU.mult)
            bp = ps.get()
            kb.mm(bp, blk2, t8)
            kb.tt(t8, bp, vu, ALU.mult)
            ps.put(bp)
            BV = t8
            kb.tt(al, kk, al, ALU.mult)
            VB = b16.get()
            kb.acopy(VB, vu)
            f32.put(vu)
            E = f32.get()
            RT, BB, KBb, AT, BT, KT = [b16.get() for _ in range(6)]
            kb.act(E, cpos, AF.Exp, scale=-1.0)
            kb.tt(RT, ru, E, ALU.mult)
            f32.put(ru)
            kb.act(E, cpos, AF.Exp)
            kb.tt(BB, al, E, ALU.mult)
            kb.tt(KBb, ku, E, ALU.mult)
            kb.tt(e1, cpos, e1, ALU.subtract)
            kb.act(E, e1, AF.Exp, scale=-1.0)
            kb.stt(AT, kk, -1.0, E, ALU.mult, ALU.mult)
            cl = cpos[:, 63::64]
            gC = small[:, 40 + r * 8: 48 + r * 8]
            kb.act(gC, cl, AF.Exp, scale=-1.0)
            kb.tt(e1.re("p (n c) -> p n c", c=64), cpos.re("p (n c) -> p n c", c=64),
                  Tl(cl.ap.unsqueeze(2).broadcast_to([128, 8, 64]), cpos.key), ALU.subtract)
            kb.act(E, e1, AF.Exp)
            kb.tt(BT, al, E, ALU.mult)
            kb.tt(KT, ku, E, ALU.mult)
            f32.put(E, e1, cpos, al, kk, ku)
            kb.mark('rw%d.loop' % r)
            RPs, YP, GPs = f32.get(), f32.get(), f32.get()
            Hst = Tl(st_t[:, 4 + r, :], ("st", 4 + r))

            def rw_tt(tt):
                tsl = slice(tt * 128, (tt + 1) * 128)
                ttbd = Tl(ttbd_t[:, 2 * (tt % 2):2 * (tt % 2) + 2, :], ("ttbd", tt % 2))
                ptk = ps.get()
                for i, src in enumerate((VB, AT, BT, KT)):
                    kb.mm(ptk[:, i * 128:(i + 1) * 128], src[:, tsl], identb)
                tok = b16.get()
                kb.acopy(tok, ptk)
                ps.put(ptk)
                tokV, tokA, tokB, tokK = [tok[:, i * 128:(i + 1) * 128] for i in range(4)]
                yield
                PA = [ps.get(), ps.get()]
                PB = [ps.get(), ps.get()]
                AS1, AS2, MM = b16.get(), b16.get(), b16.get()
                AS1v = AS1.re("p (k h t) -> p k h t", k=2, h=2)
                AS2v = AS2.re("p (k h t) -> p k h t", k=2, h=2)
                for hh in range(2):
                    hs_ = slice(hh * 64, (hh + 1) * 64)
                    kb.mm(PA[hh][:, 0:128], BB[hs_, tsl], AT[hs_, tsl])
                    kb.mm(PA[hh][:, 128:256], KBb[hs_, tsl], AT[hs_, tsl])
                    kb.mm(PA[hh][:, 256:384], BB[hs_, tsl], RT[hs_, tsl])
                    kb.mm(PA[hh][:, 384:512], KBb[hs_, tsl], RT[hs_, tsl])
                    kb.mm(PB[hh][:, 0:128], AT[hs_, tsl], BB[hs_, tsl])
                    kb.tt(AS1v[:, :, hh, :], PA[hh][:, 0:256].re("p (k t) -> p k t", k=2), bc2(m_su), ALU.mult)
                    kb.tt(AS2v[:, :, hh, :], PA[hh][:, 256:512].re("p (k t) -> p k t", k=2), bc2(m_iu), ALU.mult)
                    kb.tt(MM[:, hh * 128:(hh + 1) * 128], PB[hh][:, 0:128], m_sl, ALU.mult)
                kb.vcopy(MM[:, 256:512], AS1[:, 0:256])
                ps.put(*PA)
                ps.put(*PB)
                XT = b16.get()
                kb.tt(XT[:, 0:256].re("p (b t) -> p b t", b=2), AS1[:, 0:256].re("p (b t) -> p b t", b=2), bc2(identb), ALU.add)
                xcur = 0
                IM = b16.get()
                yield
                for j in range(5):
                    pm_ = ps.get()
                    for hh in range(2):
                        Mh = MM[:, hh * 128:(hh + 1) * 128]
                        MTh = MM[:, 256 + hh * 128:256 + (hh + 1) * 128]
                        kb.mm(pm_[:, hh * 128:(hh + 1) * 128], MTh, Mh)
                        kb.mm(pm_[:, 256 + hh * 128:256 + (hh + 1) * 128], Mh, MTh)
                    kb.tt(IM[:, 0:256].re("p (b t) -> p b t", b=2), pm_[:, 0:256].re("p (b t) -> p b t", b=2), bc2(identb), ALU.add)
                    if j < 4:
                        MM2 = b16.get()
                        kb.acopy(MM2, pm_)
                        b16.put(MM)
                        MM = MM2
                    ps.put(pm_)
                    px = ps.get()
                    for hh in range(2):
                        kb.mm(px[:, hh * 128:(hh + 1) * 128], IM[:, hh * 128:(hh + 1) * 128],
                              XT[:, xcur * 256 + hh * 128: xcur * 256 + (hh + 1) * 128])
                    xcur = 1 - xcur
                    kb.acopy(XT[:, xcur * 256: xcur * 256 + 256], px[:, 0:256])
                    ps.put(px)
                    yield
                b16.put(MM, IM)
                XTf = XT[:, xcur * 256: xcur * 256 + 256]
                pa = ps.get()
                for hh in range(2):
                    hs_ = slice(hh * 64, (hh + 1) * 64)
                    kb.mm(pa[:, hs_], AS1[:, 256 + hh * 128:256 + (hh + 1) * 128], tokV[:, hs_])
                Z = b16.get()
                Zv = Z[:, 0:256].re("p (h c) -> p h c", h=2)
                kb.acopy(Zv[:, :, 0:64], pa[:, 0:128].re("p (h c) -> p h c", h=2))
                kb.vcopy(Zv[:, :, 64:128], tokA.re("p (h c) -> p h c", h=2))
                ps.put(pa)
                yield
                pw = ps.get()
                for hh in range(2):
                    kb.mm(pw[:, hh * 128:(hh + 1) * 128], XTf[:, hh * 128:(hh + 1) * 128], Z[:, hh * 128:(hh + 1) * 128])
                WA = Z[:, 256:512]
                kb.acopy(WA, pw[:, 0:256])
                ps.put(pw)
                b16.put(XT)
                yield
                pr = ps.get()
                for hh in range(2):
                    hs_ = slice(hh * 64, (hh + 1) * 64)
                    kb.mm(pr[hs_, 0:128], WA[:, hh * 128 + 64:(hh + 1) * 128], AS2[:, hh * 128:(hh + 1) * 128])
                    kb.mm(pr[hs_, 128:256], WA[:, hh * 128:hh * 128 + 64], AS2[:, hh * 128:(hh + 1) * 128], start=True, stop=False)
                    kb.mm(pr[hs_, 128:256], tokV[:, hs_], AS2[:, 256 + hh * 128:256 + (hh + 1) * 128], start=False, stop=True)
                kb.tt(RPs[:, tsl], pr[:, 0:128], RT[:, tsl], ALU.add)
                kb.acopy(YP[:, tsl], pr[:, 128:256])
                ps.put(pr)
                b16.put(AS1, AS2)
                yield
                pe_ = [ps.get(), ps.get()]
                for c in range(2):
                    cs = slice(c * 64, (c + 1) * 64)
                    n = tt * 2 + c
                    for hh in range(2):
                        hs_ = slice(hh * 64, (hh + 1) * 64)
                        kb.mm(pe_[c][hs_, hh * 64:(hh + 1) * 64], WA[cs, hh * 128 + 64:(hh + 1) * 128], tokB[cs, hs_])
                        kb.mm(pe_[c][hs_, 128:192], tokB[cs, hs_], WA[cs, hh * 128:hh * 128 + 64], start=True, stop=False)
                        kb.mm(pe_[c][hs_, 128:192], tokK[cs, hs_], tokV[cs, hs_], start=False, stop=True)
                    kb.vcopy(GPs[:, n * 64:(n + 1) * 64], pe_[c][:, 128:192])
                    for hh in range(2):
                        hs_ = slice(hh * 64, (hh + 1) * 64)
                        kb.stt(ttbd[hs_, c, hh * 64:(hh + 1) * 64], ident[hs_, hh * 64:(hh + 1) * 64], gC[hs_, n:n + 1],
                               pe_[c][hs_, hh * 64:(hh + 1) * 64], ALU.mult, ALU.add)
                ps.put(*pe_)
                b16.put(tok, Z)
                yield
                py = [ps.get(), ps.get()]
                for c in range(2):
                    n = tt * 2 + c
                    ns = slice(n * 64, (n + 1) * 64)
                    for hh in range(2):
                        hs_ = slice(hh * 64, (hh + 1) * 64)
                        kb.mm(py[hh][hs_, c * 64:(c + 1) * 64], Hst[hs_, :], RPs[hs_, ns])
                    ph = ps.get()
                    kb.mm(ph[:, 0:64], ttbd[:, c, :], Hst)
                    kb.tt(Hst, ph[:, 0:64], GPs[:, ns], ALU.add)
                    ps.put(ph)
                for hh in range(2):
                    hs_ = slice(hh * 64, (hh + 1) * 64)
                    kb.tt(YP[hs_, tsl], py[hh][hs_, 0:128], YP[hs_, tsl], ALU.add)
                ps.put(*py)

            gens = [rw_tt(tt) for tt in range(NT)]
            lru_gen = lru_pair(l, s, hT, ycat, r) if "nolru" not in kb.dbg else None
            active, nxt = [], 0
            while nxt < NT or active:
                if nxt < NT and len(active) < 2 and (not active or active[-1][1] >= 2):
                    active.append([gens[nxt], 0])
                    nxt += 1
                for a_ in list(active):
                    try:
                        next(a_[0])
                        a_[1] += 1
                    except StopIteration:
                        active.remove(a_)
                if lru_gen is not None:
                    try:
                        next(lru_gen)
                    except StopIteration:
                        lru_gen = None
            while lru_gen is not None:
                try:
                    next(lru_gen)
                except StopIteration:
                    lru_gen = None
            b16.put(VB, RT, BB, KBb, AT, BT, KT)
            kb.mark('rw%d.fin' % r)
            Y = RPs
            kb.vcopy(Y, YP)
            if l == 0:
                kb.dump_tile("rw_y", Y, 128, s * SEG, SEG, T, r0=r * 128, total_rows=256)
            kb.act(YP, Y, AF.Square)
            p1, p2 = ps.get(), ps.get()
            kb.mm(p1, blk2, Y)
            kb.mm(p2, blk2, YP)
            mean = GPs
            kb.act(mean, p1, AF.Copy, scale=1.0 / 64)
            kb.act(YP, p1, AF.Square, scale=1.0 / 64)
            kb.stt(YP, p2, 1.0 / 64, YP, ALU.mult, ALU.subtract)
            ps.put(p1, p2)
            kb.rsqrt(YP, YP, bias=GN_EPS)
            kb.tt(Y, Y, mean, ALU.subtract)
            kb.tt(Y, Y, YP, ALU.mult)
            kb.ts(Y, Y, col("ln_w", r), col("ln_b", r), ALU.mult, ALU.add)
            kb.tt(Y, Y, BV, ALU.add)
            gp = ps.get()
            kb.mm(gp, pmat[64:128, rs], Lw[64:128, :])
            ycat[r] = b16.get()
            kb.tt(ycat[r], Y, gp, ALU.mult)
            ps.put(gp)
            f32.put(RPs, YP, GPs, BV)
        f32.put(Lw)

    def layer_setup(l):
        kb.dma("sp", pcol[:, 0:NPCOL_IN], pcol_d[l], "pcol")
        kb.dma("sp", pmat, pmat_d[l], "pmat")
        for dt in range(8):
            for pc, c0 in enumerate(range(0, WCOLS, 1024)):
                c1 = min(WCOLS, c0 + 1024)
                kb.dma("pool", win[dt][:, c0:c1], win_d[l, dt, :, c0:c1], ("win", dt), nosync=(pc > 0))
        c = lambda n: pcol[:, PCOL[n][0]:PCOL[n][0] + PCOL[n][1]]
        kb.ts(c("omu"), c("mu"), -1.0, 1.0, ALU.mult, ALU.add)
        kb.ts(c("nw0"), c("w0"), -1.0, None, ALU.mult)
        kb.ts(c("omka"), c("k_a"), -1.0, 1.0, ALU.mult, ALU.add)
        kb.ts(c("ngla_b"), c("gla_b"), -1.0, None, ALU.mult)
        kb.act(c("lc"), c("lam"), AF.Softplus, scale=-1.0)
        kb.ts(c("lc2"), c("lc"), -16.0, None, ALU.mult)
        kb.ts(c("lc"), c("lc"), -8.0, None, ALU.mult)
        tmp = c("tmp")
        kb.act(tmp, c("lb_logits"), AF.Exp)
        for r in range(2):
            t4 = tmp[:, r * 4:(r + 1) * 4]
            sm = small[:, 32 + r:33 + r]
            kb.tt(sm, t4[:, 0:1], t4[:, 1:2], ALU.add)
            kb.tt(sm, sm, t4[:, 2:3], ALU.add)
            kb.tt(sm, sm, t4[:, 3:4], ALU.add)
            kb.recip(sm, sm)
            lbc = col("lb", r)
            kb.memset(lbc, 0.0)
            for j in range(1, l + 1):
                kb.tt(lbc, lbc, t4[:, j:j + 1], ALU.add)
            kb.tt(lbc, lbc, sm, ALU.mult)
            kb.ts(col("omlb", r), lbc, -1.0, 1.0, ALU.mult, ALU.add)
        kb.memset(carry, 0.0)
        kb.memset(Tl(ttbd_t[:, 0:2, :], ("ttbd", 0)), 0.0)
        kb.memset(Tl(ttbd_t[:, 2:4, :], ("ttbd", 1)), 0.0)
        for i in range(6):
            kb.memset(Tl(st_t[:, i, :], ("st", i)), 0.0)

    def segment(l, s):
        kb.mark('L%d.S%d.norm' % (l, s))
        hT = rms_in(s, "g_mix_pre")
        if l == 0:
            for dt in range(8):
                kb.dump_tile("hT", hT[dt], 128, s * SEG, SEG, T, r0=dt * 128, total_rows=1024)
        ycat = [None] * 8
        for nm, fn, i0 in (("rwkv", mixer_rwkv, 0), ("gla", mixer_gla, 2), ("lru", mixer_lru, 4), ("hgrn", mixer_hgrn, 6)):
            kb.mark("L%d.S%d.%s" % (l, s, nm))
            if nm == "lru" and "norwkv" not in kb.dbg and "nolru" not in kb.dbg:
                continue
            if nm != "rwkv":
                ycat[i0], ycat[i0 + 1] = b16.get(), b16.get()
            if ("no" + nm) in kb.dbg:
                if ycat[i0] is None:
                    ycat[i0], ycat[i0 + 1] = b16.get(), b16.get()
                kb.memset(ycat[i0], 0.0)
                kb.memset(ycat[i0 + 1], 0.0)
            else:
                fn(l, s, hT, ycat)
        b16.put(*hT)
        if l == 0:
            for i in range(8):
                kb.dump_tile("ycat", ycat[i], 128, s * SEG, SEG, T, r0=i * 128, total_rows=1024)
        kb.mark('L%d.S%d.wout' % (l, s))
        mt, sqs = [], []
        for fo in range(8):
            wc = next_chunk()
            p = ps.get()
            for dt in range(8):
                kb.mm(p, wc[:, dt * 128:(dt + 1) * 128], ycat[dt], start=(dt == 0), stop=(dt == 7))
            m = f32.get()
            kb.acopy(m, p)
            sq = b16.get()
            kb.act(sq, p, AF.Square)
            ps.put(p)
            mt.append(m)
            sqs.append(sq)
        b16.put(*ycat)
        post_norm_residual(s, mt, sqs, "g_mix_post")
        if l == 0:
            for dt in range(8):
                kb.dump_tile("xmid", xT[dt][s], 128, s * SEG, SEG, T, r0=dt * 128, total_rows=1024)
        if "noffn" in kb.dbg:
            for _ in range(2 * NJ + 24):
                next_chunk()
            return
        kb.mark('L%d.S%d.ffn' % (l, s))
        h2 = rms_in(s, "g_ffn_pre")
        hid = []
        for j in range(NJ):
            wg = next_chunk()
            wu = next_chunk()
            pg = ps.get()
            pu = ps.get()
            for dt in range(8):
                kb.mm(pg, wg[:, dt * 128:(dt + 1) * 128], h2[dt], start=(dt == 0), stop=(dt == 7))
            for dt in range(8):
                kb.mm(pu, wu[:, dt * 128:(dt + 1) * 128], h2[dt], start=(dt == 0), stop=(dt == 7))
            sg = f32.get()
            kb.act(sg, pg, AF.Silu)
            ps.put(pg)
            hj = b16.get()
            kb.tt(hj, sg, pu, ALU.mult)
            ps.put(pu)
            f32.put(sg)
            hid.append(hj)
        b16.put(*h2)
        mt, sqs = [], []
        for fo in range(8):
            p = ps.get()
            pieces = ((0, 8), (8, 16), (16, 22))
            for pc, (j0, j1) in enumerate(pieces):
                wc = next_chunk()
                for j in range(j0, j1):
                    kb.mm(p, wc[:, (j - j0) * 128:(j - j0 + 1) * 128], hid[j], start=(j == 0), stop=(j == NJ - 1))
            m = f32.get()
            kb.acopy(m, p)
            sq = b16.get()
            kb.act(sq, p, AF.Square)
            ps.put(p)
            mt.append(m)
            sqs.append(sq)
        b16.put(*hid)
        post_norm_residual(s, mt, sqs, "g_ffn_post")

    kb.mixer_rwkv_slot = [mixer_rwkv]
    kb.env = dict(locals())
    for l in range(depth):
        layer_setup(l)
        for s in range(NSEG):
            segment(l, s)
    for dt in range(8):
        for s in range(NSEG):
            kb.dma("sp", out_d[dt * 128:(dt + 1) * 128, s * SEG:(s + 1) * SEG], xT[dt][s], "out")
    S.emit()
    st.close()
    kb.n_ops = len(S.ops)
    return nc, kb


_CACHE = {}


def kernel(**inputs):
    x = np.asarray(inputs["x"], np.float32)
    B = x.shape[0]
    if "nc" not in _CACHE:
        _CACHE["nc"] = build(L)[0]
    nc = _CACHE["nc"]
    lay = [_host_layout(inputs, l) for l in range(L)]
    win = np.stack([a[0] for a in lay])
    wch = np.stack([a[1] for a in lay])
    pcol = np.stack([a[2] for a in lay])
    pmat = np.stack([a[3] for a in lay])
    consts = _host_consts()
    in_maps = []
    for b in range(B):
        in_maps.append({"xT": np.ascontiguousarray(x[b].T), "win": win, "wch": wch, "pcol": pcol, "pmat": pmat,
                        "consts": consts})
    res = run_bass_kernel_spmd(nc, in_maps, core_ids=list(range(B)))
    out = np.stack([np.asarray(r["outT"]).T for r in res.results]).astype(np.float32)
    return out
```

```python
import contextlib
import numpy as np
import concourse.bass as bass
import concourse.mybir as mybir
from concourse.bass_utils import run_bass_kernel_spmd

F32 = mybir.dt.float32
BF16 = mybir.dt.bfloat16
AF = mybir.ActivationFunctionType
ALU = mybir.AluOpType

D = 1024
T = 2048
L = 4
SEG = 512
NSEG = T // SEG
NT = SEG // 128
FFN = 2816
NJ = FFN // 128
EPS = 1e-6
GN_EPS = 64e-5

RW_R, RW_K, RW_V, RW_LOW = 0, 256, 512, 768
GL_Q, GL_K, GL_V, GL_A, GL_G = 896, 1152, 1408, 1664, 1680
LR_X, LR_G = 1936, 2192
HG_Q, HG_F, HG_I, HG_G = 2448, 2704, 2960, 3216
WCOLS = 3472
NCHL = 8 + 2 * NJ + 24

QUEUES = ("pe", "act", "dve", "pool", "sp")


class Sched:
    def __init__(self, nc, same_engine_sync=True):
        self.nc = nc
        self.ops = []
        self.last_w = {}
        self.readers = {}
        self.same_engine_sync = same_engine_sync

    def add(self, eng, fn, reads=(), writes=(), dma=None, nosync=False):
        idx = len(self.ops)
        deps = {}
        if not nosync:
            for k in reads:
                w = self.last_w.get(k)
                if w is not None:
                    deps[w] = True
                if isinstance(k, tuple) and k[0] == "ps":
                    for r in self.readers.get(k, ()):
                        if self.ops[r]["eng"] != eng:
                            deps.setdefault(r, False)
            for k in writes:
                w = self.last_w.get(k)
                if w is not None:
                    deps.setdefault(w, False)
                for r in self.readers.get(k, ()):
                    deps.setdefault(r, False)
        self.ops.append(dict(eng=eng, fn=fn, deps=deps, dma=dma))
        for k in reads:
            lst = self.readers.setdefault(k, [])
            if dma is None:
                lst[:] = [r for r in lst if not (self.ops[r]["dma"] is None and self.ops[r]["eng"] == eng)]
            lst.append(idx)
        for k in writes:
            self.last_w[k] = idx
            self.readers[k] = []
        return idx

    def emit(self, final_wait_eng="sp"):
        nc = self.nc
        ops = self.ops
        needed = set()
        for o in ops:
            needed |= set(o["deps"])
        last = {}
        for i, o in enumerate(ops):
            if o["dma"] is None:
                last[o["eng"]] = i
            else:
                last[("dma", o["dma"])] = i
        needed |= set(last.values())
        val, cnt, semkey_of = {}, {}, {}
        for i, o in enumerate(ops):
            if o["dma"] is not None:
                sk = ("dma", o["dma"])
                cnt[sk] = cnt.get(sk, 0) + 16
            else:
                sk = ("eng", o["eng"])
                if i in needed:
                    cnt[sk] = cnt.get(sk, 0) + 1
            val[i] = cnt.get(sk, 0)
            semkey_of[i] = sk
        totals = dict(cnt)
        stack = contextlib.ExitStack()
        sems = {}
        for n, sk in enumerate(totals):
            sems[sk] = stack.enter_context(nc.semaphore("s%d" % n))
        self.n_sems = len(sems)
        per_eng = {q: [] for q in QUEUES}
        for i, o in enumerate(ops):
            per_eng[o["eng"]].append(i)

        def wait_list(i, known):
            o = ops[i]
            need = {}
            for d, is_raw in o["deps"].items():
                od = ops[d]
                sk = semkey_of[d]
                if od["dma"] is None and o["dma"] is None and od["eng"] == o["eng"]:
                    if od["eng"] == "pe" or not self.same_engine_sync:
                        continue
                    if not is_raw:
                        self.n_relaxed += 1
                        continue
                v = val[d]
                if v > need.get(sk, 0):
                    need[sk] = v
            out = []
            for sk, v in need.items():
                if known.get(sk, 0) >= v:
                    continue
                known[sk] = v
                out.append((sk, v))
            return out

        block = stack.enter_context(nc.Block())
        handles = dict(pe="tensor", act="scalar", dve="vector", pool="gpsimd", sp="sync")
        self.n_waits = 0
        self.n_relaxed = 0

        def make_body(q):
            def body(e):
                known = {}
                for i in per_eng[q]:
                    o = ops[i]
                    for sk, v in wait_list(i, known):
                        e.wait_ge(sems[sk], v)
                        self.n_waits += 1
                    ins = o["fn"](e)
                    if o["dma"] is not None:
                        ins.then_inc(sems[semkey_of[i]], 16)
                    elif i in needed:
                        ins.then_inc(sems[semkey_of[i]], 1)
                if q == final_wait_eng:
                    for sk, v in totals.items():
                        if known.get(sk, 0) < v:
                            e.wait_ge(sems[sk], v)
            return body

        for q in QUEUES:
            if per_eng[q] or q == final_wait_eng:
                getattr(block, handles[q])(make_body(q))
        stack.close()


class Tl:
    __slots__ = ("ap", "key")

    def __init__(self, ap, key):
        self.ap = ap
        self.key = key

    def __getitem__(self, idx):
        return Tl(self.ap[idx], self.key)

    def re(self, s, **kw):
        return Tl(self.ap.rearrange(s, **kw), self.key)


def _ap(x):
    return x.ap if isinstance(x, Tl) else x


def _keys(*xs):
    return [x.key for x in xs if isinstance(x, Tl)]


class Pool:
    def __init__(self, tiles):
        self.free = list(tiles)

    def get(self):
        return self.free.pop(0)

    def put(self, *ts):
        for t in ts:
            self.free.append(t)


class KB:
    def __init__(self, nc, depth, dbg):
        self.nc = nc
        self.depth = depth
        self.dbg = dbg or ()
        self.S = Sched(nc)
        self.dumps = {}
        self.marks = []

    def act(self, out, in_, func, scale=1.0, bias=0.0):
        kw = {}
        self.S.add("act", lambda e: e.activation(out=_ap(out), in_=_ap(in_), func=func, scale=_ap(scale), bias=_ap(bias)),
                   reads=_keys(in_, scale, bias), writes=_keys(out))

    def tt(self, out, a, b, op):
        self.S.add("dve", lambda e: e.tensor_tensor(out=_ap(out), in0=_ap(a), in1=_ap(b), op=op),
                   reads=_keys(a, b), writes=_keys(out))

    def ts(self, out, a, s1, s2, op0, op1=None):
        if op1 is None:
            self.S.add("dve", lambda e: e.tensor_scalar(out=_ap(out), in0=_ap(a), scalar1=_ap(s1), scalar2=None, op0=op0),
                       reads=_keys(a, s1), writes=_keys(out))
        else:
            self.S.add("dve", lambda e: e.tensor_scalar(out=_ap(out), in0=_ap(a), scalar1=_ap(s1), scalar2=_ap(s2), op0=op0, op1=op1),
                       reads=_keys(a, s1, s2), writes=_keys(out))

    def stt(self, out, a, s, b, op0, op1):
        self.S.add("dve", lambda e: e.scalar_tensor_tensor(out=_ap(out), in0=_ap(a), scalar=_ap(s), in1=_ap(b), op0=op0, op1=op1),
                   reads=_keys(a, s, b), writes=_keys(out))

    def scan(self, out, d0, d1, init):
        self.S.add("dve", lambda e: e.tensor_tensor_scan(out=_ap(out), data0=_ap(d0), data1=_ap(d1), initial=_ap(init), op0=ALU.mult, op1=ALU.add),
                   reads=_keys(d0, d1, init), writes=_keys(out))

    def rsqrt(self, out, in_, scale=1.0, bias=0.0):
        self.act(out, in_, AF.Ln, scale=scale, bias=bias)
        self.act(out, out, AF.Exp, scale=-0.5)

    def recip(self, out, in_):
        self.S.add("dve", lambda e: e.reciprocal(out=_ap(out), in_=_ap(in_)), reads=_keys(in_), writes=_keys(out))

    def vcopy(self, out, in_):
        self.S.add("dve", lambda e: e.tensor_copy(out=_ap(out), in_=_ap(in_)), reads=_keys(in_), writes=_keys(out))

    def acopy(self, out, in_):
        self.act(out, in_, AF.Copy)

    def memset(self, out, v, eng="dve"):
        self.S.add(eng, lambda e: e.memset(_ap(out), v), writes=_keys(out))

    def mm(self, out, lhsT, rhs, start=True, stop=True):
        self.S.add("pe", lambda e: e.matmul(_ap(out), lhsT=_ap(lhsT), rhs=_ap(rhs), start=start, stop=stop),
                   reads=_keys(lhsT, rhs), writes=_keys(out))

    def tr(self, out, in_, ident):
        self.S.add("pe", lambda e: e.transpose(out=_ap(out), in_=_ap(in_), identity=_ap(ident)),
                   reads=_keys(in_, ident), writes=_keys(out))

    def dma(self, q, out, in_, semkey, nosync=False):
        self.S.add(q, lambda e: e.dma_start(out=_ap(out), in_=_ap(in_)), reads=_keys(in_), writes=_keys(out),
                   dma=semkey, nosync=nosync)

    def mark(self, name):
        self.marks.append((name, sum(1 for o in self.S.ops if o['eng'] == 'pe'), sum(1 for o in self.S.ops if o['eng'] == 'dve')))

    def dump(self, name, t, shape):
        if name not in self.dbg:
            return
        d = self.nc.dram_tensor("dbg_" + name, list(shape), F32, kind="ExternalOutput").ap()
        self.dumps[name] = d
        return d

    def dump_tile(self, name, t, rows, c0, ncols, total_cols, r0=0, total_rows=None):
        if name not in self.dbg:
            return
        if name not in self.dumps:
            self.dumps[name] = self.nc.dram_tensor("dbg_" + name, [total_rows or rows, total_cols], F32, kind="ExternalOutput").ap()
        d = self.dumps[name]
        if t.ap.dtype != F32:
            tmp = self.f32.get()
            self.vcopy(tmp[0:rows, 0:ncols], t)
            self.dma("sp", d[r0:r0 + rows, c0:c0 + ncols], tmp[0:rows, 0:ncols], "out")
            self.f32.put(tmp)
        else:
            self.dma("sp", d[r0:r0 + rows, c0:c0 + ncols], t, "out")


PCOL = {}


def _pcol_layout():
    names = []

    def add(n, k):
        PCOL[n] = (sum(x[1] for x in names), k)
        names.append((n, k))
    add("g_mix_pre", 8); add("g_mix_post", 8); add("g_ffn_pre", 8); add("g_ffn_post", 8)
    add("mu", 7); add("w0", 2); add("a0", 2); add("k_k", 2); add("k_a", 2); add("r_k", 2); add("ln_w", 2); add("ln_b", 2)
    add("gla_b", 2); add("gla_nw", 1)
    add("conv_w", 8); add("conv_b", 2); add("b_a", 2); add("b_x", 2); add("lam", 2)
    add("lb_logits", 8); add("hg_nw", 1)
    add("omu", 7); add("nw0", 2); add("omka", 2); add("ngla_b", 2); add("lc", 2); add("lc2", 2); add("lb", 2); add("omlb", 2)
    add("tmp", 8)
    return sum(x[1] for x in names)


NPCOL = _pcol_layout()
NPCOL_IN = PCOL["omu"][0]


def _host_layout(inp, l):
    f = np.float32
    w_in = np.asarray(inp["w_in"][l], f)
    arr = np.zeros((D, WCOLS), f)
    arr[:, 0:896] = w_in[:, 0:896]
    g0 = 896
    for nm, dst in (("q", GL_Q), ("k", GL_K)):
        src = g0 + (0 if nm == "q" else 128)
        for h in range(4):
            arr[:, dst + h * 64: dst + h * 64 + 32] = w_in[:, src + h * 32: src + (h + 1) * 32]
    arr[:, GL_V:GL_V + 256] = w_in[:, g0 + 256: g0 + 512]
    arr[:, GL_A:GL_A + 16] = w_in[:, g0 + 512: g0 + 528]
    arr[:, GL_G:GL_G + 256] = w_in[:, g0 + 528: g0 + 784]
    arr[:, LR_X:LR_X + 512] = w_in[:, 1680:2192]
    arr[:, HG_Q:HG_Q + 1024] = w_in[:, 2192:3216]
    win_h = arr.reshape(8, 128, WCOLS)

    ch = np.zeros((NCHL, 128, 1024), f)
    w_out = np.asarray(inp["w_out"][l], f)
    for fo in range(8):
        ch[fo] = w_out[:, fo * 128:(fo + 1) * 128].reshape(8, 128, 128).transpose(1, 0, 2).reshape(128, 1024)
    wgu = np.asarray(inp["ffn_w_gate_up"][l], f)
    for j in range(NJ):
        ch[8 + 2 * j] = wgu[:, j * 128:(j + 1) * 128].reshape(8, 128, 128).transpose(1, 0, 2).reshape(128, 1024)
        ch[8 + 2 * j + 1] = wgu[:, FFN + j * 128: FFN + (j + 1) * 128].reshape(8, 128, 128).transpose(1, 0, 2).reshape(128, 1024)
    wd = np.asarray(inp["ffn_w_down"][l], f)
    for fo in range(8):
        blk = wd[:, fo * 128:(fo + 1) * 128].reshape(NJ, 128, 128).transpose(1, 0, 2)
        for pc, (j0, j1) in enumerate(((0, 8), (8, 16), (16, 22))):
            ch[8 + 2 * NJ + fo * 3 + pc][:, 0:(j1 - j0) * 128] = blk[:, j0:j1, :].reshape(128, (j1 - j0) * 128)

    pc = np.zeros((128, NPCOL_IN), f)

    def put(nm, vec):
        vec = np.asarray(vec, f).reshape(-1)
        c0, k = PCOL[nm]
        assert vec.size == 128 * k, (nm, vec.size, k)
        pc[:, c0:c0 + k] = vec.reshape(k, 128).T
    put("g_mix_pre", inp["norm_mix_pre"][l]); put("g_mix_post", inp["norm_mix_post"][l])
    put("g_ffn_pre", inp["norm_ffn_pre"][l]); put("g_ffn_post", inp["norm_ffn_post"][l])
    put("mu", inp["rwkv_shift_mu"][l])
    for nm, key in (("w0", "rwkv_w0"), ("a0", "rwkv_a0"), ("k_k", "rwkv_k_k"), ("k_a", "rwkv_k_a"), ("r_k", "rwkv_r_k"),
                    ("ln_w", "rwkv_ln_w"), ("ln_b", "rwkv_ln_b"), ("conv_b", "lru_conv_b"), ("b_a", "lru_b_a"),
                    ("b_x", "lru_b_x"), ("lam", "lru_lambda")):
        put(nm, inp[key][l])
    gb = np.zeros(256, f)
    gbs = np.asarray(inp["gla_gate_b"][l], f)
    for h in range(4):
        gb[h * 64:h * 64 + 32] = gbs[h * 32:(h + 1) * 32]
    put("gla_b", gb)
    put("gla_nw", np.tile(np.asarray(inp["gla_norm_w"][l], f), 2))
    put("hg_nw", np.tile(np.asarray(inp["hgrn_norm_w"][l], f), 2))
    cw = np.asarray(inp["lru_conv_w"][l], f)
    put("conv_w", np.stack([cw[:, 0:128], cw[:, 128:256]], 0).reshape(-1))
    lbl = np.asarray(inp["hgrn_lb_logits"], f)
    put("lb_logits", np.stack([lbl[:, 0:128], lbl[:, 128:256]], 0).reshape(-1))

    pm = np.zeros((128, 1024), f)
    pm[0:32, 0:256] = inp["rwkv_w_up"][l]
    pm[32:64, 0:256] = inp["rwkv_a_up"][l]
    pm[64:128, 0:256] = inp["rwkv_g_up"][l]
    gu = np.asarray(inp["gla_gate_up"][l], f)
    for h in range(4):
        pm[0:16, 256 + h * 64: 256 + h * 64 + 32] = gu[:, h * 32:(h + 1) * 32]
    wa = np.asarray(inp["lru_w_a"][l], f)
    wx = np.asarray(inp["lru_w_x"][l], f)
    for r in range(2):
        for hh in range(2):
            pm[hh * 64:(hh + 1) * 64, 512 + r * 128 + hh * 64: 512 + r * 128 + (hh + 1) * 64] = wa[2 * r + hh]
            pm[hh * 64:(hh + 1) * 64, 768 + r * 128 + hh * 64: 768 + r * 128 + (hh + 1) * 64] = wx[2 * r + hh]
    return win_h, ch, pc, pm


NCONST = 128 * 6 + 4 + 512 * 2


def _host_consts():
    c = np.zeros((128, NCONST), np.float32)
    i = np.arange(128)
    c[:, 0:128] = np.eye(128)
    c[:, 128:256] = (i[:, None] // 64 == i[None, :] // 64)
    same64 = (i[:, None] // 64 == i[None, :] // 64)
    c[:, 256:384] = same64 & (i[:, None] < i[None, :])
    c[:, 384:512] = same64 & (i[:, None] > i[None, :])
    c[:, 512:640] = same64 & (i[:, None] <= i[None, :])
    same32 = (i[:, None] // 32 == i[None, :] // 32)
    c[:, 640:768] = same32 & (i[:, None] <= i[None, :])
    for n in range(4):
        c[:, 768 + n] = (i // 32 == n)
    t = np.arange(512)
    c[:, 772:772 + 512] = (t % 32 != 0)[None, :]
    c[:, 772 + 512:772 + 1024] = (t % 64 != 0)[None, :]
    return c


def build(depth=L, dbg=None, dbg_stop=None):
    nc = bass.Bass("TRN2", target_bir_lowering=False)
    kb = KB(nc, depth, dbg)
    S = kb.S
    x_d = nc.dram_tensor("xT", [D, T], F32, kind="ExternalInput").ap()
    win_d = nc.dram_tensor("win", [depth, 8, 128, WCOLS], F32, kind="ExternalInput").ap()
    wch_d = nc.dram_tensor("wch", [depth, NCHL, 128, 1024], F32, kind="ExternalInput").ap()
    pcol_d = nc.dram_tensor("pcol", [depth, 128, NPCOL_IN], F32, kind="ExternalInput").ap()
    pmat_d = nc.dram_tensor("pmat", [depth, 128, 1024], F32, kind="ExternalInput").ap()
    const_d = nc.dram_tensor("consts", [128, NCONST], F32, kind="ExternalInput").ap()
    out_d = nc.dram_tensor("outT", [D, T], F32, kind="ExternalOutput").ap()

    st = contextlib.ExitStack()

    def sb(name, shape, dt):
        return st.enter_context(nc.sbuf_tensor(name, shape, dt))

    xT_t = sb("xT_sb", [128, 8, T], F32)
    win_t = sb("win_sb", [128, 8, WCOLS], BF16)
    pcol_t = sb("pcol_sb", [128, NPCOL], F32)
    pmat_t = sb("pmat_sb", [128, 1024], F32)
    cst_t = sb("cst_sb", [128, NCONST], F32)
    cstb_t = sb("cstb_sb", [128, 768], BF16)
    NA = 6
    wA_t = sb("wA_sb", [128, NA, 1024], BF16)
    NF32, NB16 = 12, 36
    f32_t = sb("f32pool", [128, NF32, 512], F32)
    b16_t = sb("b16pool", [128, NB16, 512], BF16)
    carry_t = sb("carry_sb", [128, 7 + 2 + 2 * 3], F32)
    st_t = sb("state_sb", [128, 6, 64], F32)
    ttbd_t = sb("ttbd_sb", [128, 4, 128], F32)
    small_t = sb("small_sb", [128, 64], F32)
    psf = [st.enter_context(nc.psum_tensor("ps%d" % i, [128, 512], F32)) for i in range(8)]

    xT = [[Tl(xT_t[:, dt, s * SEG:(s + 1) * SEG], ("x", dt, s)) for s in range(NSEG)] for dt in range(8)]
    win = [Tl(win_t[:, dt, :], ("win", dt)) for dt in range(8)]
    pcol = Tl(pcol_t[:], "pcol")
    pmat = Tl(pmat_t[:], "pmat")
    cst = Tl(cst_t[:], "cst")
    cstb = Tl(cstb_t[:], "cstb")
    kb.f32 = Pool([Tl(f32_t[:, i, :], ("f32", i)) for i in range(NF32)])
    kb.b16 = Pool([Tl(b16_t[:, i, :], ("b16", i)) for i in range(NB16)])
    kb.ps = Pool([Tl(psf[i][:], ("ps", i)) for i in range(8)])
    carry = Tl(carry_t[:], "carry")
    small = Tl(small_t[:], "small")
    f32, b16, ps = kb.f32, kb.b16, kb.ps

    ident = cst[:, 0:128]
    blk2 = cst[:, 128:256]
    mask4 = cst[:, 768:772]
    cmask32 = cst[:, 772:772 + 512]
    cmask64 = cst[:, 772 + 512:772 + 1024]
    identb = cstb[:, 0:128]
    onesb = cstb[:, 128:256]
    m_su, m_sl, m_iu, m_iu32 = cstb[:, 256:384], cstb[:, 384:512], cstb[:, 512:640], cstb[:, 640:768]

    def col(name, j=0):
        c0, k = PCOL[name]
        return pcol[:, c0 + j:c0 + j + 1]

    kb.dma("sp", cst, const_d[:, :], "cst")
    kb.dma("pool", cstb, const_d[:, 0:768], "cstb")
    for s in range(NSEG):
        S.add("sp", (lambda s: lambda e: e.dma_start(out=xT_t[:, :, s * SEG:(s + 1) * SEG],
                                                      in_=x_d[:, s * SEG:(s + 1) * SEG].rearrange("(dt p) t -> p dt t", p=128)))(s),
              writes=[("x", dt, s) for dt in range(8)], dma=("xin", s))
    allones = Tl(sb("allones", [128, 128], BF16)[:], "allones")
    kb.memset(allones, 1.0)

    wq = []
    for l in range(depth):
        for s in range(NSEG):
            for c in range(NCHL):
                wq.append((l, c))
    wstate = dict(issued=0, used=0)

    def next_chunk():
        i = wstate["used"]
        while wstate["issued"] < min(len(wq), i + NA - 1):
            j = wstate["issued"]
            lj, cj = wq[j]
            slot = j % NA
            kb.dma("pool", Tl(wA_t[:, slot, :], ("wA", slot)), wch_d[lj, cj], ("wA", slot))
            wstate["issued"] += 1
        wstate["used"] += 1
        return Tl(wA_t[:, i % NA, :], ("wA", i % NA))

    def norm_stats_rstd(sq_list_fn, n, inv_n, eps, lhs):
        ssp = ps.get()
        for i in range(n):
            sq = sq_list_fn(i)
            kb.mm(ssp, lhs, sq, start=(i == 0), stop=(i == n - 1))
            b16.put(sq)
        rstd = f32.get()
        kb.rsqrt(rstd, ssp, scale=inv_n, bias=eps)
        ps.put(ssp)
        return rstd

    def rms_in(s, gname):
        def sqf(dt):
            sq = b16.get()
            kb.act(sq, xT[dt][s], AF.Square)
            return sq
        rstd = norm_stats_rstd(sqf, 8, 1.0 / D, EPS, allones)
        hs = []
        for dt in range(8):
            h = b16.get()
            kb.stt(h, xT[dt][s], col(gname, dt), rstd, ALU.mult, ALU.mult)
            hs.append(h)
        f32.put(rstd)
        return hs

    def proj(hT, c0, m):
        p = ps.get()
        for dt in range(8):
            kb.mm(p[0:m, :], win[dt][:, c0:c0 + m], hT[dt], start=(dt == 0), stop=(dt == 7))
        return p

    def proj_tok(hT, c0, n, tt, p):
        for dt in range(8):
            kb.mm(p, hT[dt][:, tt * 128:(tt + 1) * 128], win[dt][:, c0:c0 + n], start=(dt == 0), stop=(dt == 7))

    def post_norm_residual(s, mtiles, sqs, gname):
        it = iter(sqs)
        rstd = norm_stats_rstd(lambda i: next(it), 8, 1.0 / D, EPS, allones)
        for fo in range(8):
            kb.stt(mtiles[fo], mtiles[fo], col(gname, fo), rstd, ALU.mult, ALU.mult)
            kb.tt(xT[fo][s], xT[fo][s], mtiles[fo], ALU.add)
            f32.put(mtiles[fo])
        f32.put(rstd)

    def gla_core(hT, r, q_ps, k_sb, lg, vcol, gcol, nwcol, qscale, Scar, yout, tag, l, s):
        cum = f32.get()
        kb.scan(cum, cmask32, lg, 0.0)
        f32.put(lg)
        e = f32.get()
        kb.act(e, cum, AF.Exp)
        qe = b16.get()
        kb.stt(qe, q_ps, qscale, e, ALU.mult, ALU.mult)
        ps.put(q_ps)
        kb.act(e, cum, AF.Exp, scale=-1.0)
        ke = b16.get()
        kb.tt(ke, k_sb, e, ALU.mult)
        cl = cum[:, 31::32]
        an = small[:, r * 16:(r + 1) * 16]
        kb.act(an, cl, AF.Exp)
        kb.tt(e.re("p (n c) -> p n c", c=32), Tl(cl.ap.unsqueeze(2).broadcast_to([128, 16, 32]), cum.key),
              cum.re("p (n c) -> p n c", c=32), ALU.subtract)
        kb.act(e, e, AF.Exp)
        kl = f32.get()
        kb.tt(kl, k_sb, e, ALU.mult)
        f32.put(e, cum, k_sb)
        o_sb = f32.get()
        kb.mark('%s%d.p1' % (tag, r))
        Gs = [f32.get(), f32.get()]
        atts = [b16.get(), b16.get()]
        vtoks = b16.get()
        klTs = b16.get()
        vms = [b16.get() for _ in range(NT)]
        TS = [slice(tt * 128, (tt + 1) * 128) for tt in range(NT)]
        for tt in range(NT):
            apb = [ps.get(), ps.get()]
            att = atts[tt // 2][:, (tt % 2) * 256:(tt % 2) * 256 + 256]
            for hh in range(2):
                hs_ = slice(hh * 64, (hh + 1) * 64)
                kb.mm(apb[hh][:, 0:128], ke[hs_, TS[tt]], qe[hs_, TS[tt]])
                kb.tt(att[:, hh * 128:(hh + 1) * 128], apb[hh][:, 0:128], m_iu32, ALU.mult)
            ps.put(*apb)
        for tt in range(NT):
            tp = ps.get()
            kb.mm(tp[:, 0:128], kl[:, TS[tt]], ident)
            kb.acopy(klTs[:, TS[tt]], tp[:, 0:128])
            ps.put(tp)
            vp = ps.get()
            proj_tok(hT, vcol, 128, tt, vp[:, 0:128])
            kb.tt(vms[tt].re("p (n v) -> p n v", n=4), Tl(vp.ap[:, 0:128].unsqueeze(1).broadcast_to([128, 4, 128]), vp.key),
                  Tl(mask4.ap.unsqueeze(2).broadcast_to([128, 4, 128]), mask4.key), ALU.mult)
            kb.vcopy(vtoks[:, TS[tt]], vp[:, 0:128])
            ps.put(vp)
        for tt in range(NT):
            gp = ps.get()
            for hh in range(2):
                hs_ = slice(hh * 64, (hh + 1) * 64)
                for n in range(4):
                    kb.mm(gp[hs_, n * 64:(n + 1) * 64], klTs[:, tt * 128 + hh * 64: tt * 128 + (hh + 1) * 64],
                          vms[tt][:, n * 128 + hh * 64: n * 128 + (hh + 1) * 64])
            kb.acopy(Gs[tt // 2][:, (tt % 2) * 256:(tt % 2) * 256 + 256], gp[:, 0:256])
            ps.put(gp)
        for tt in range(NT):
            att = atts[tt // 2][:, (tt % 2) * 256:(tt % 2) * 256 + 256]
            opb = [ps.get(), ps.get()]
            for hh in range(2):
                hs_ = slice(hh * 64, (hh + 1) * 64)
                kb.mm(opb[hh][hs_, 0:128], vtoks[:, tt * 128 + hh * 64: tt * 128 + (hh + 1) * 64], att[:, hh * 128:(hh + 1) * 128])
                kb.acopy(o_sb[hs_, TS[tt]], opb[hh][hs_, 0:128])
            ps.put(*opb)
        b16.put(klTs, *vms)
        f32.put(kl)
        b16.put(ke, vtoks, *atts)
        kb.mark('%s%d.p2' % (tag, r))
        Sch = [f32.get(), f32.get()]
        kb.vcopy(Sch[0][:, 0:64], Scar)
        for n in range(16):
            src = Sch[n // 8][:, (n % 8) * 64:(n % 8) * 64 + 64]
            dst = Sch[(n + 1) // 8][:, ((n + 1) % 8) * 64:((n + 1) % 8) * 64 + 64] if n < 15 else Scar
            kb.stt(dst, src, an[:, n:n + 1], Gs[n // 8][:, (n % 8) * 64:(n % 8) * 64 + 64], ALU.mult, ALU.add)
        sbf = [b16.get(), b16.get()]
        kb.acopy(sbf[0], Sch[0])
        kb.acopy(sbf[1], Sch[1])
        f32.put(*Gs)
        f32.put(*Sch)
        kb.mark('%s%d.p3' % (tag, r))
        for tt in range(NT):
            tsl = slice(tt * 128, (tt + 1) * 128)
            opb = [ps.get(), ps.get()]
            for hh in range(2):
                hs_ = slice(hh * 64, (hh + 1) * 64)
                for n in range(4):
                    c = tt * 4 + n
                    kb.mm(opb[hh][hs_, n * 32:(n + 1) * 32], sbf[c // 8][hs_, (c % 8) * 64:(c % 8) * 64 + 64],
                          qe[hs_, tt * 128 + n * 32: tt * 128 + (n + 1) * 32])
                kb.tt(o_sb[hs_, tsl], opb[hh][hs_, 0:128], o_sb[hs_, tsl], ALU.add)
            ps.put(*opb)
        b16.put(qe, *sbf)
        kb.dump_tile(tag + "_o", o_sb, 128, s * SEG, SEG, T, r0=r * 128, total_rows=256) if l == 0 else None
        kb.mark('%s%d.fin' % (tag, r))
        osq = f32.get()
        kb.act(osq, o_sb, AF.Square)
        ssp = ps.get()
        kb.mm(ssp, blk2, osq)
        kb.rsqrt(osq, ssp, scale=1.0 / 64, bias=EPS)
        ps.put(ssp)
        kb.stt(o_sb, o_sb, nwcol, osq, ALU.mult, ALU.mult)
        gp = proj(hT, gcol, 128)
        kb.act(osq, gp, AF.Silu)
        ps.put(gp)
        kb.tt(yout, o_sb, osq, ALU.mult)
        f32.put(osq, o_sb)

    def mixer_hgrn(l, s, hT, ycat):
        for r in range(2):
            q_ps = proj(hT, HG_Q + r * 128, 128)
            fp = proj(hT, HG_F + r * 128, 128)
            f = f32.get()
            kb.act(f, fp, AF.Sigmoid)
            ps.put(fp)
            kb.ts(f, f, col("omlb", r), col("lb", r), ALU.mult, ALU.add)
            k = f32.get()
            kb.ts(k, f, -1.0, 1.0, ALU.mult, ALU.add)
            kb.act(f, f, AF.Ln)
            gla_core(hT, r, q_ps, k, f, HG_I + r * 128, HG_G + r * 128, col("hg_nw"), 1.0,
                     Tl(st_t[:, 2 + r, :], ("st", 2 + r)), ycat[6 + r], "hg", l, s)

    def mixer_gla(l, s, hT, ycat):
        alp = proj(hT, GL_A, 16)
        alow = f32.get()
        kb.acopy(alow[0:16, :], alp[0:16, :])
        ps.put(alp)
        for r in range(2):
            q_ps = proj(hT, GL_Q + r * 128, 128)
            kp = proj(hT, GL_K + r * 128, 128)
            k = f32.get()
            kb.acopy(k, kp)
            ps.put(kp)
            mp = ps.get()
            kb.mm(mp, pmat[0:16, 256 + r * 128: 256 + (r + 1) * 128], alow[0:16, :])
            lg = f32.get()
            kb.act(lg, mp, AF.Softplus, scale=-1.0, bias=col("ngla_b", r))
            ps.put(mp)
            kb.ts(lg, lg, -1.0 / 16.0, None, ALU.mult)
            gla_core(hT, r, q_ps, k, lg, GL_V + r * 128, GL_G + r * 128, col("gla_nw"), 32.0 ** -0.5,
                     Tl(st_t[:, r, :], ("st", r)), ycat[2 + r], "gl", l, s)
        f32.put(alow)

    def lru_pair(l, s, hT, ycat, r):
        if True:
            xp = proj(hT, LR_X + r * 128, 128)
            tail = carry[:, 9 + 3 * r: 12 + 3 * r]
            cw = lambda kk_: col("conv_w", r * 4 + kk_)
            xc = f32.get()
            kb.ts(xc, xp, cw(3), col("conv_b", r), ALU.mult, ALU.add)
            for kk_ in range(3):
                sh = 3 - kk_
                kb.stt(xc[:, sh:SEG], xp[:, 0:SEG - sh], cw(kk_), xc[:, sh:SEG], ALU.mult, ALU.add)
                kb.stt(xc[:, 0:sh], tail[:, 3 - sh:3], cw(kk_), xc[:, 0:sh], ALU.mult, ALU.add)
            kb.vcopy(tail, xp[:, SEG - 3:SEG])
            ps.put(xp)
            yield
            gap = ps.get()
            kb.mm(gap, pmat[:, 512 + r * 128: 512 + (r + 1) * 128], xc)
            rg = f32.get()
            kb.act(rg, gap, AF.Sigmoid, bias=col("b_a", r))
            ps.put(gap)
            gxp = ps.get()
            kb.mm(gxp, pmat[:, 768 + r * 128: 768 + (r + 1) * 128], xc)
            ig = f32.get()
            kb.act(ig, gxp, AF.Sigmoid, bias=col("b_x", r))
            ps.put(gxp)
            yield
            a = f32.get()
            kb.act(a, rg, AF.Exp, scale=col("lc", r))
            kb.act(rg, rg, AF.Exp, scale=col("lc2", r))
            kb.act(rg, rg, AF.Sqrt, scale=-1.0, bias=1.0)
            kb.tt(ig, ig, xc, ALU.mult)
            kb.tt(ig, ig, rg, ALU.mult)
            yield
            hc = carry[:, 7 + r: 8 + r]
            kb.scan(xc, a, ig, hc)
            kb.vcopy(hc, xc[:, SEG - 1:SEG])
            yield
            gp = proj(hT, LR_G + r * 128, 128)
            kb.act(rg, gp, AF.Gelu_apprx_tanh)
            ps.put(gp)
            kb.tt(ycat[4 + r], xc, rg, ALU.mult)
            f32.put(xc, rg, ig, a)

    def mixer_lru(l, s, hT, ycat):
        gens = [lru_pair(l, s, hT, ycat, 0), lru_pair(l, s, hT, ycat, 1)]
        while gens:
            for g in list(gens):
                try:
                    next(g)
                except StopIteration:
                    gens.remove(g)

    def shift(p_ps, mu_i, out):
        cc = carry[:, mu_i:mu_i + 1]
        kb.ts(out, p_ps, col("omu", mu_i), None, ALU.mult)
        kb.stt(out[:, 1:SEG], p_ps[:, 0:SEG - 1], col("mu", mu_i), out[:, 1:SEG], ALU.mult, ALU.add)
        kb.stt(out[:, 0:1], cc, col("mu", mu_i), out[:, 0:1], ALU.mult, ALU.add)
        kb.vcopy(cc, p_ps[:, SEG - 1:SEG])
        ps.put(p_ps)

    def bc2(t):
        return Tl(t.ap.unsqueeze(1).broadcast_to([128, 2, 128]), t.key)

    def bc4(t):
        return Tl(t.ap.unsqueeze(1).broadcast_to([128, 4, 128]), t.key)

    def mixer_rwkv(l, s, hT, ycat):
        Lw = f32.get()
        shift(proj(hT, RW_LOW, 128), 6, Lw)
        kb.act(Lw[0:32, :], Lw[0:32, :], AF.Tanh)
        kb.act(Lw[64:128, :], Lw[64:128, :], AF.Sigmoid)
        for r in range(2):
            rs = slice(r * 128, (r + 1) * 128)
            ru, ku, vu = f32.get(), f32.get(), f32.get()
            shift(proj(hT, RW_R + r * 128, 128), 0 + r, ru)
            shift(proj(hT, RW_K + r * 128, 128), 2 + r, ku)
            shift(proj(hT, RW_V + r * 128, 128), 4 + r, vu)
            wp = ps.get()
            kb.mm(wp, pmat[0:32, rs], Lw[0:32, :])
            e1 = f32.get()
            kb.act(e1, wp, AF.Softplus, scale=-1.0, bias=col("nw0", r))
            ps.put(wp)
            kb.act(e1, e1, AF.Exp, scale=-1.0, bias=-0.5)
            cpos = f32.get()
            kb.scan(cpos, cmask64, e1, 0.0)
            ap_ = ps.get()
            kb.mm(ap_, pmat[32:64, rs], Lw[32:64, :])
            al = f32.get()
            kb.act(al, ap_, AF.Sigmoid, bias=col("a0", r))
            ps.put(ap_)
            kk = f32.get()
            kb.ts(kk, ku, col("k_k", r), None, ALU.mult)
            t8 = f32.get()
            kb.act(t8, kk, AF.Square)
            sp_ = ps.get()
            kb.mm(sp_, blk2, t8)
            kb.rsqrt(t8, sp_, bias=1e-16)
            ps.put(sp_)
            kb.tt(kk, kk, t8, ALU.mult)
            kb.ts(t8, al, col("k_a", r), col("omka", r), ALU.mult, ALU.add)
            kb.tt(ku, ku, t8, ALU.mult)
            kb.tt(t8, ru, ku, ALU.mult)
            kb.ts(t8, t8, col("r_k", r), None, ALU.mult)
            bp = ps.get()
            kb.mm(bp, blk2, t8)
            kb.tt(t8, bp, vu, ALU.mult)
            ps.put(bp)
            BV = t8
            kb.tt(al, kk, al, ALU.mult)
            VB = b16.get()
            kb.acopy(VB, vu)
            f32.put(vu)
            E = f32.get()
            RT, BB, KBb, AT, BT, KT = [b16.get() for _ in range(6)]
            kb.act(E, cpos, AF.Exp, scale=-1.0)
            kb.tt(RT, ru, E, ALU.mult)
            f32.put(ru)
            kb.act(E, cpos, AF.Exp)
            kb.tt(BB, al, E, ALU.mult)
            kb.tt(KBb, ku, E, ALU.mult)
            kb.tt(e1, cpos, e1, ALU.subtract)
            kb.act(E, e1, AF.Exp, scale=-1.0)
            kb.stt(AT, kk, -1.0, E, ALU.mult, ALU.mult)
            cl = cpos[:, 63::64]
            gC = small[:, 40 + r * 8: 48 + r * 8]
            kb.act(gC, cl, AF.Exp, scale=-1.0)
            kb.tt(e1.re("p (n c) -> p n c", c=64), cpos.re("p (n c) -> p n c", c=64),
                  Tl(cl.ap.unsqueeze(2).broadcast_to([128, 8, 64]), cpos.key), ALU.subtract)
            kb.act(E, e1, AF.Exp)
            kb.tt(BT, al, E, ALU.mult)
            kb.tt(KT, ku, E, ALU.mult)
            f32.put(E, e1, cpos, al, kk, ku)
            kb.mark('rw%d.loop' % r)
            RPs, YP, GPs = f32.get(), f32.get(), f32.get()
            Hst = Tl(st_t[:, 4 + r, :], ("st", 4 + r))

            def rw_tt(tt):
                tsl = slice(tt * 128, (tt + 1) * 128)
                ttbd = Tl(ttbd_t[:, 2 * (tt % 2):2 * (tt % 2) + 2, :], ("ttbd", tt % 2))
                ptk = ps.get()
                for i, src in enumerate((VB, AT, BT, KT)):
                    kb.mm(ptk[:, i * 128:(i + 1) * 128], src[:, tsl], identb)
                tok = b16.get()
                kb.acopy(tok, ptk)
                ps.put(ptk)
                tokV, tokA, tokB, tokK = [tok[:, i * 128:(i + 1) * 128] for i in range(4)]
                yield
                PA = [ps.get(), ps.get()]
                PB = [ps.get(), ps.get()]
                AS1, AS2, MM = b16.get(), b16.get(), b16.get()
                AS1v = AS1.re("p (k h t) -> p k h t", k=2, h=2)
                AS2v = AS2.re("p (k h t) -> p k h t", k=2, h=2)
                for hh in range(2):
                    hs_ = slice(hh * 64, (hh + 1) * 64)
                    kb.mm(PA[hh][:, 0:128], BB[hs_, tsl], AT[hs_, tsl])
                    kb.mm(PA[hh][:, 128:256], KBb[hs_, tsl], AT[hs_, tsl])
                    kb.mm(PA[hh][:, 256:384], BB[hs_, tsl], RT[hs_, tsl])
                    kb.mm(PA[hh][:, 384:512], KBb[hs_, tsl], RT[hs_, tsl])
                    kb.mm(PB[hh][:, 0:128], AT[hs_, tsl], BB[hs_, tsl])
                    kb.tt(AS1v[:, :, hh, :], PA[hh][:, 0:256].re("p (k t) -> p k t", k=2), bc2(m_su), ALU.mult)
                    kb.tt(AS2v[:, :, hh, :], PA[hh][:, 256:512].re("p (k t) -> p k t", k=2), bc2(m_iu), ALU.mult)
                    kb.tt(MM[:, hh * 128:(hh + 1) * 128], PB[hh][:, 0:128], m_sl, ALU.mult)
                kb.vcopy(MM[:, 256:512], AS1[:, 0:256])
                ps.put(*PA)
                ps.put(*PB)
                XT = b16.get()
                kb.tt(XT[:, 0:256].re("p (b t) -> p b t", b=2), AS1[:, 0:256].re("p (b t) -> p b t", b=2), bc2(identb), ALU.add)
                xcur = 0
                IM = b16.get()
                yield
                for j in range(5):
                    pm_ = ps.get()
                    for hh in range(2):
                        Mh = MM[:, hh * 128:(hh + 1) * 128]
                        MTh = MM[:, 256 + hh * 128:256 + (hh + 1) * 128]
                        kb.mm(pm_[:, hh * 128:(hh + 1) * 128], MTh, Mh)
                        kb.mm(pm_[:, 256 + hh * 128:256 + (hh + 1) * 128], Mh, MTh)
                    kb.tt(IM[:, 0:256].re("p (b t) -> p b t", b=2), pm_[:, 0:256].re("p (b t) -> p b t", b=2), bc2(identb), ALU.add)
                    if j < 4:
                        MM2 = b16.get()
                        kb.acopy(MM2, pm_)
                        b16.put(MM)
                        MM = MM2
                    ps.put(pm_)
                    px = ps.get()
                    for hh in range(2):
                        kb.mm(px[:, hh * 128:(hh + 1) * 128], IM[:, hh * 128:(hh + 1) * 128],
                              XT[:, xcur * 256 + hh * 128: xcur * 256 + (hh + 1) * 128])
                    xcur = 1 - xcur
                    kb.acopy(XT[:, xcur * 256: xcur * 256 + 256], px[:, 0:256])
                    ps.put(px)
                    yield
                b16.put(MM, IM)
                XTf = XT[:, xcur * 256: xcur * 256 + 256]
                pa = ps.get()
                for hh in range(2):
                    hs_ = slice(hh * 64, (hh + 1) * 64)
                    kb.mm(pa[:, hs_], AS1[:, 256 + hh * 128:256 + (hh + 1) * 128], tokV[:, hs_])
                Z = b16.get()
                Zv = Z[:, 0:256].re("p (h c) -> p h c", h=2)
                kb.acopy(Zv[:, :, 0:64], pa[:, 0:128].re("p (h c) -> p h c", h=2))
                kb.vcopy(Zv[:, :, 64:128], tokA.re("p (h c) -> p h c", h=2))
                ps.put(pa)
                yield
                pw = ps.get()
                for hh in range(2):
                    kb.mm(pw[:, hh * 128:(hh + 1) * 128], XTf[:, hh * 128:(hh + 1) * 128], Z[:, hh * 128:(hh + 1) * 128])
                WA = Z[:, 256:512]
                kb.acopy(WA, pw[:, 0:256])
                ps.put(pw)
                b16.put(XT)
                yield
                pr = ps.get()
                for hh in range(2):
                    hs_ = slice(hh * 64, (hh + 1) * 64)
                    kb.mm(pr[hs_, 0:128], WA[:, hh * 128 + 64:(hh + 1) * 128], AS2[:, hh * 128:(hh + 1) * 128])
                    kb.mm(pr[hs_, 128:256], WA[:, hh * 128:hh * 128 + 64], AS2[:, hh * 128:(hh + 1) * 128], start=True, stop=False)
                    kb.mm(pr[hs_, 128:256], tokV[:, hs_], AS2[:, 256 + hh * 128:256 + (hh + 1) * 128], start=False, stop=True)
                kb.tt(RPs[:, tsl], pr[:, 0:128], RT[:, tsl], ALU.add)
                kb.acopy(YP[:, tsl], pr[:, 128:256])
                ps.put(pr)
                b16.put(AS1, AS2)
                yield
                pe_ = [ps.get(), ps.get()]
                for c in range(2):
                    cs = slice(c * 64, (c + 1) * 64)
                    n = tt * 2 + c
                    for hh in range(2):
                        hs_ = slice(hh * 64, (hh + 1) * 64)
                        kb.mm(pe_[c][hs_, hh * 64:(hh + 1) * 64], WA[cs, hh * 128 + 64:(hh + 1) * 128], tokB[cs, hs_])
                        kb.mm(pe_[c][hs_, 128:192], tokB[cs, hs_], WA[cs, hh * 128:hh * 128 + 64], start=True, stop=False)
                        kb.mm(pe_[c][hs_, 128:192], tokK[cs, hs_], tokV[cs, hs_], start=False, stop=True)
                    kb.vcopy(GPs[:, n * 64:(n + 1) * 64], pe_[c][:, 128:192])
                    for hh in range(2):
                        hs_ = slice(hh * 64, (hh + 1) * 64)
                        kb.stt(ttbd[hs_, c, hh * 64:(hh + 1) * 64], ident[hs_, hh * 64:(hh + 1) * 64], gC[hs_, n:n + 1],
                               pe_[c][hs_, hh * 64:(hh + 1) * 64], ALU.mult, ALU.add)
                ps.put(*pe_)
                b16.put(tok, Z)
                yield
                py = [ps.get(), ps.get()]
                for c in range(2):
                    n = tt * 2 + c
                    ns = slice(n * 64, (n + 1) * 64)
                    for hh in range(2):
                        hs_ = slice(hh * 64, (hh + 1) * 64)
                        kb.mm(py[hh][hs_, c * 64:(c + 1) * 64], Hst[hs_, :], RPs[hs_, ns])
                    ph = ps.get()
                    kb.mm(ph[:, 0:64], ttbd[:, c, :], Hst)
                    kb.tt(Hst, ph[:, 0:64], GPs[:, ns], ALU.add)
                    ps.put(ph)
                for hh in range(2):
                    hs_ = slice(hh * 64, (hh + 1) * 64)
                    kb.tt(YP[hs_, tsl], py[hh][hs_, 0:128], YP[hs_, tsl], ALU.add)
                ps.put(*py)

            gens = [rw_tt(tt) for tt in range(NT)]
            active, nxt = [], 0
            while nxt < NT or active:
                if nxt < NT and len(active) < 2 and (not active or active[-1][1] >= 2):
                    active.append([gens[nxt], 0])
                    nxt += 1
                for a_ in list(active):
                    try:
                        next(a_[0])
                        a_[1] += 1
                    except StopIteration:
                        active.remove(a_)
            b16.put(VB, RT, BB, KBb, AT, BT, KT)
            kb.mark('rw%d.fin' % r)
            Y = RPs
            kb.vcopy(Y, YP)
            if l == 0:
                kb.dump_tile("rw_y", Y, 128, s * SEG, SEG, T, r0=r * 128, total_rows=256)
            kb.act(YP, Y, AF.Square)
            p1, p2 = ps.get(), ps.get()
            kb.mm(p1, blk2, Y)
            kb.mm(p2, blk2, YP)
            mean = GPs
            kb.act(mean, p1, AF.Copy, scale=1.0 / 64)
            kb.act(YP, p1, AF.Square, scale=1.0 / 64)
            kb.stt(YP, p2, 1.0 / 64, YP, ALU.mult, ALU.subtract)
            ps.put(p1, p2)
            kb.rsqrt(YP, YP, bias=GN_EPS)
            kb.tt(Y, Y, mean, ALU.subtract)
            kb.tt(Y, Y, YP, ALU.mult)
            kb.ts(Y, Y, col("ln_w", r), col("ln_b", r), ALU.mult, ALU.add)
            kb.tt(Y, Y, BV, ALU.add)
            gp = ps.get()
            kb.mm(gp, pmat[64:128, rs], Lw[64:128, :])
            ycat[r] = b16.get()
            kb.tt(ycat[r], Y, gp, ALU.mult)
            ps.put(gp)
            f32.put(RPs, YP, GPs, BV)
        f32.put(Lw)

    def layer_setup(l):
        kb.dma("sp", pcol[:, 0:NPCOL_IN], pcol_d[l], "pcol")
        kb.dma("sp", pmat, pmat_d[l], "pmat")
        for dt in range(8):
            for pc, c0 in enumerate(range(0, WCOLS, 1024)):
                c1 = min(WCOLS, c0 + 1024)
                kb.dma("pool", win[dt][:, c0:c1], win_d[l, dt, :, c0:c1], ("win", dt), nosync=(pc > 0))
        c = lambda n: pcol[:, PCOL[n][0]:PCOL[n][0] + PCOL[n][1]]
        kb.ts(c("omu"), c("mu"), -1.0, 1.0, ALU.mult, ALU.add)
        kb.ts(c("nw0"), c("w0"), -1.0, None, ALU.mult)
        kb.ts(c("omka"), c("k_a"), -1.0, 1.0, ALU.mult, ALU.add)
        kb.ts(c("ngla_b"), c("gla_b"), -1.0, None, ALU.mult)
        kb.act(c("lc"), c("lam"), AF.Softplus, scale=-1.0)
        kb.ts(c("lc2"), c("lc"), -16.0, None, ALU.mult)
        kb.ts(c("lc"), c("lc"), -8.0, None, ALU.mult)
        tmp = c("tmp")
        kb.act(tmp, c("lb_logits"), AF.Exp)
        for r in range(2):
            t4 = tmp[:, r * 4:(r + 1) * 4]
            sm = small[:, 32 + r:33 + r]
            kb.tt(sm, t4[:, 0:1], t4[:, 1:2], ALU.add)
            kb.tt(sm, sm, t4[:, 2:3], ALU.add)
            kb.tt(sm, sm, t4[:, 3:4], ALU.add)
            kb.recip(sm, sm)
            lbc = col("lb", r)
            kb.memset(lbc, 0.0)
            for j in range(1, l + 1):
                kb.tt(lbc, lbc, t4[:, j:j + 1], ALU.add)
            kb.tt(lbc, lbc, sm, ALU.mult)
            kb.ts(col("omlb", r), lbc, -1.0, 1.0, ALU.mult, ALU.add)
        kb.memset(carry, 0.0)
        kb.memset(Tl(ttbd_t[:, 0:2, :], ("ttbd", 0)), 0.0)
        kb.memset(Tl(ttbd_t[:, 2:4, :], ("ttbd", 1)), 0.0)
        for i in range(6):
            kb.memset(Tl(st_t[:, i, :], ("st", i)), 0.0)

    def segment(l, s):
        kb.mark('L%d.S%d.norm' % (l, s))
        hT = rms_in(s, "g_mix_pre")
        if l == 0:
            for dt in range(8):
                kb.dump_tile("hT", hT[dt], 128, s * SEG, SEG, T, r0=dt * 128, total_rows=1024)
        ycat = [None] * 8
        for nm, fn, i0 in (("rwkv", mixer_rwkv, 0), ("gla", mixer_gla, 2), ("lru", mixer_lru, 4), ("hgrn", mixer_hgrn, 6)):
            kb.mark("L%d.S%d.%s" % (l, s, nm))
            if nm != "rwkv":
                ycat[i0], ycat[i0 + 1] = b16.get(), b16.get()
            if ("no" + nm) in kb.dbg:
                if ycat[i0] is None:
                    ycat[i0], ycat[i0 + 1] = b16.get(), b16.get()
                kb.memset(ycat[i0], 0.0)
                kb.memset(ycat[i0 + 1], 0.0)
            else:
                fn(l, s, hT, ycat)
        b16.put(*hT)
        if l == 0:
            for i in range(8):
                kb.dump_tile("ycat", ycat[i], 128, s * SEG, SEG, T, r0=i * 128, total_rows=1024)
        kb.mark('L%d.S%d.wout' % (l, s))
        mt, sqs = [], []
        for fo in range(8):
            wc = next_chunk()
            p = ps.get()
            for dt in range(8):
                kb.mm(p, wc[:, dt * 128:(dt + 1) * 128], ycat[dt], start=(dt == 0), stop=(dt == 7))
            m = f32.get()
            kb.acopy(m, p)
            sq = b16.get()
            kb.act(sq, p, AF.Square)
            ps.put(p)
            mt.append(m)
            sqs.append(sq)
        b16.put(*ycat)
        post_norm_residual(s, mt, sqs, "g_mix_post")
        if l == 0:
            for dt in range(8):
                kb.dump_tile("xmid", xT[dt][s], 128, s * SEG, SEG, T, r0=dt * 128, total_rows=1024)
        if "noffn" in kb.dbg:
            for _ in range(2 * NJ + 24):
                next_chunk()
            return
        kb.mark('L%d.S%d.ffn' % (l, s))
        h2 = rms_in(s, "g_ffn_pre")
        hid = []
        for j in range(NJ):
            wg = next_chunk()
            wu = next_chunk()
            pg = ps.get()
            pu = ps.get()
            for dt in range(8):
                kb.mm(pg, wg[:, dt * 128:(dt + 1) * 128], h2[dt], start=(dt == 0), stop=(dt == 7))
            for dt in range(8):
                kb.mm(pu, wu[:, dt * 128:(dt + 1) * 128], h2[dt], start=(dt == 0), stop=(dt == 7))
            sg = f32.get()
            kb.act(sg, pg, AF.Silu)
            ps.put(pg)
            hj = b16.get()
            kb.tt(hj, sg, pu, ALU.mult)
            ps.put(pu)
            f32.put(sg)
            hid.append(hj)
        b16.put(*h2)
        mt, sqs = [], []
        for fo in range(8):
            p = ps.get()
            pieces = ((0, 8), (8, 16), (16, 22))
            for pc, (j0, j1) in enumerate(pieces):
                wc = next_chunk()
                for j in range(j0, j1):
                    kb.mm(p, wc[:, (j - j0) * 128:(j - j0 + 1) * 128], hid[j], start=(j == 0), stop=(j == NJ - 1))
            m = f32.get()
            kb.acopy(m, p)
            sq = b16.get()
            kb.act(sq, p, AF.Square)
            ps.put(p)
            mt.append(m)
            sqs.append(sq)
        b16.put(*hid)
        post_norm_residual(s, mt, sqs, "g_ffn_post")

    kb.mixer_rwkv_slot = [mixer_rwkv]
    kb.env = dict(locals())
    for l in range(depth):
        layer_setup(l)
        for s in range(NSEG):
            segment(l, s)
    for dt in range(8):
        for s in range(NSEG):
            kb.dma("sp", out_d[dt * 128:(dt + 1) * 128, s * SEG:(s + 1) * SEG], xT[dt][s], "out")
    S.emit()
    st.close()
    kb.n_ops = len(S.ops)
    return nc, kb


_CACHE = {}


def kernel(**inputs):
    x = np.asarray(inputs["x"], np.float32)
    B = x.shape[0]
    if "nc" not in _CACHE:
        _CACHE["nc"] = build(L)[0]
    nc = _CACHE["nc"]
    lay = [_host_layout(inputs, l) for l in range(L)]
    win = np.stack([a[0] for a in lay])
    wch = np.stack([a[1] for a in lay])
    pcol = np.stack([a[2] for a in lay])
    pmat = np.stack([a[3] for a in lay])
    consts = _host_consts()
    in_maps = []
    for b in range(B):
        in_maps.append({"xT": np.ascontiguousarray(x[b].T), "win": win, "wch": wch, "pcol": pcol, "pmat": pmat,
                        "consts": consts})
    res = run_bass_kernel_spmd(nc, in_maps, core_ids=list(range(B)))
    out = np.stack([np.asarray(r["outT"]).T for r in res.results]).astype(np.float32)
    return out
```
